# Optimizing a Trainium2 kernel written in Bass

```python
import math
import jax, jax.numpy as jnp
from jax import lax
import numpy as np

D_MODEL = 1024
BATCH = 2
SEQ = 8192
DEPTH = 2

N_EVEN = (DEPTH + 1) // 2
N_ODD = DEPTH // 2

S5_WIDTH = D_MODEL // 2
S5_GROUP = 16
S5_GROUPS = S5_WIDTH // S5_GROUP
S5_STATE = 64
S5_MIN_STEP = 0.001
S5_MAX_STEP = 0.1
SB_HEAD_DIM = 64
SB_HEADS = (D_MODEL // 2) // SB_HEAD_DIM
SB_WIDTH = SB_HEADS * SB_HEAD_DIM
EVEN_IN = S5_WIDTH + 3 * SB_WIDTH
EVEN_MIX = S5_WIDTH + SB_WIDTH
FOX_HEAD_DIM = 64
FOX_HEADS = D_MODEL // FOX_HEAD_DIM
FOX_WIDTH = FOX_HEADS * FOX_HEAD_DIM
ODD_IN = 3 * FOX_WIDTH + FOX_HEADS
D_FF = 2816
N_EXPERTS = 8
TOP_K = 2
D_FF_EXPERT = 3584
Q_BLOCK = 128
RMS_EPS = 1e-6

kernel_name = "hybrid_s5_stickbreak_fox_moe"

F32 = jnp.float32


def rmsnorm(x, g):
    xf = x.astype(F32)
    y = xf * lax.rsqrt(jnp.mean(xf * xf, axis=-1, keepdims=True) + RMS_EPS)
    return (y * g.astype(F32)).astype(x.dtype)


def to_heads(t, n_heads, head_dim):
    b, l, _ = t.shape
    return t.reshape(b, l, n_heads, head_dim).transpose(0, 2, 1, 3)


def from_heads(t):
    b, h, l, d = t.shape
    return t.transpose(0, 2, 1, 3).reshape(b, l, h * d)


def s5_mixer(u, a_re, a_im, b_re, b_im, c_re, c_im, d_skip, log_step, w_glu, b_glu):
    bsz, seqlen, _ = u.shape
    uf = u.astype(F32).reshape(bsz, seqlen, S5_GROUPS, S5_GROUP)
    lam = lax.complex(a_re.astype(F32), a_im.astype(F32))
    step = jnp.exp(log_step.astype(F32))[:, None]
    lam_bar = jnp.exp(lam * step)
    b_cplx = lax.complex(b_re.astype(F32), b_im.astype(F32))
    b_bar = ((lam_bar - 1.0) / lam)[..., None] * b_cplx
    bu = jnp.einsum('gph,blgh->blgp', b_bar, uf.astype(jnp.complex64))
    a_seq = jnp.broadcast_to(lam_bar, bu.shape)

    def combine(left, right):
        a_l, s_l = left
        a_r, s_r = right
        return a_r * a_l, a_r * s_l + s_r

    _, states = lax.associative_scan(combine, (a_seq, bu), axis=1)
    c_cplx = lax.complex(c_re.astype(F32), c_im.astype(F32))
    y = jnp.einsum('ghp,blgp->blgh', c_cplx, states).real + d_skip.astype(F32) * uf
    y = jax.nn.gelu(y.reshape(bsz, seqlen, S5_WIDTH))
    out = y * jax.nn.sigmoid(y @ w_glu.astype(F32) + b_glu.astype(F32))
    return out.astype(u.dtype)


def stick_breaking_attention(q, k, v):
    bsz, nh, seqlen, dh = q.shape
    nblk = seqlen // Q_BLOCK
    scale = 1.0 / math.sqrt(dh)
    qb = q.reshape(bsz, nh, nblk, Q_BLOCK, dh).transpose(2, 0, 1, 3, 4)
    kpos = jnp.arange(seqlen)

    def block(args):
        qi, blk = args
        qpos = blk * Q_BLOCK + jnp.arange(Q_BLOCK)
        z = jnp.einsum('bhqd,bhkd->bhqk', qi, k).astype(F32) * scale
        strict = kpos[None, :] < qpos[:, None]
        log_keep = jnp.where(strict, jax.nn.log_sigmoid(-z), 0.0)
        after = lax.cumsum(log_keep, axis=3, reverse=True) - log_keep
        w = jnp.where(strict, jnp.exp(jax.nn.log_sigmoid(z) + after), 0.0)
        return jnp.einsum('bhqk,bhkd->bhqd', w.astype(v.dtype), v)

    out = lax.map(block, (qb, jnp.arange(nblk)))
    return out.transpose(1, 2, 0, 3, 4).reshape(bsz, nh, seqlen, dh)


def forgetting_attention(q, k, v, log_f):
    bsz, nh, seqlen, dh = q.shape
    nblk = seqlen // Q_BLOCK
    scale = 1.0 / math.sqrt(dh)
    cum = lax.cumsum(log_f, axis=2)
    qb = q.reshape(bsz, nh, nblk, Q_BLOCK, dh).transpose(2, 0, 1, 3, 4)
    cb = cum.reshape(bsz, nh, nblk, Q_BLOCK).transpose(2, 0, 1, 3)
    kpos = jnp.arange(seqlen)

    def block(args):
        qi, ci, blk = args
        qpos = blk * Q_BLOCK + jnp.arange(Q_BLOCK)
        s = jnp.einsum('bhqd,bhkd->bhqk', qi, k).astype(F32) * scale
        s = s + ci[..., None] - cum[:, :, None, :]
        causal = kpos[None, :] <= qpos[:, None]
        p = jax.nn.softmax(jnp.where(causal, s, -jnp.inf), axis=-1)
        return jnp.einsum('bhqk,bhkd->bhqd', p.astype(v.dtype), v)

    out = lax.map(block, (qb, cb, jnp.arange(nblk)))
    return out.transpose(1, 2, 0, 3, 4).reshape(bsz, nh, seqlen, dh)


def swiglu(h, w_gate, w_up, w_down):
    return (jax.nn.silu(h @ w_gate) * (h @ w_up)) @ w_down


def moe_swiglu(h, w_router, w_gate, w_up, w_down):
    logits = (h @ w_router).astype(F32)
    top_val, top_idx = lax.top_k(logits, TOP_K)
    gates = jax.nn.softmax(top_val, axis=-1)
    combine = jnp.sum(jax.nn.one_hot(top_idx, N_EXPERTS, dtype=F32) * gates[..., None], axis=-2)
    out = jnp.zeros_like(h)
    for e in range(N_EXPERTS):
        y_e = swiglu(h, w_gate[e], w_up[e], w_down[e])
        out = out + combine[..., e:e + 1].astype(h.dtype) * y_e
    return out


def setup_inputs(seed: int = 0) -> dict:
    key = jax.random.key(seed)
    ks = iter(jax.random.split(key, 40))

    def nrm(shape, scale):
        return jax.random.normal(next(ks), shape, F32) * scale

    def gain(shape):
        return 1.0 + 0.02 * jax.random.normal(next(ks), shape, F32)

    E, O = N_EVEN, N_ODD
    G, H, P = S5_GROUPS, S5_GROUP, S5_STATE
    n_idx = jnp.arange(P, dtype=F32)
    inp = {}
    inp["x"] = nrm((BATCH, SEQ, D_MODEL), 1.0)
    inp["ev_norm_mix"] = gain((E, D_MODEL))
    inp["ev_w_in"] = nrm((E, D_MODEL, EVEN_IN), D_MODEL ** -0.5)
    inp["s5_a_re"] = -0.5 + 0.01 * jax.random.normal(next(ks), (E, G, P), F32)
    inp["s5_a_im"] = math.pi * n_idx + 0.01 * jax.random.normal(next(ks), (E, G, P), F32)
    inp["s5_b_re"] = nrm((E, G, P, H), (2.0 * H) ** -0.5)
    inp["s5_b_im"] = nrm((E, G, P, H), (2.0 * H) ** -0.5)
    inp["s5_c_re"] = nrm((E, G, H, P), P ** -0.5)
    inp["s5_c_im"] = nrm((E, G, H, P), P ** -0.5)
    inp["s5_d"] = nrm((E, G, H), 1.0)
    inp["s5_log_step"] = jax.random.uniform(next(ks), (E, G), F32, math.log(S5_MIN_STEP), math.log(S5_MAX_STEP))
    inp["s5_w_glu"] = nrm((E, S5_WIDTH, S5_WIDTH), S5_WIDTH ** -0.5)
    inp["s5_b_glu"] = nrm((E, S5_WIDTH), 0.01)
    inp["ev_w_out"] = nrm((E, EVEN_MIX, D_MODEL), EVEN_MIX ** -0.5)
    inp["ev_norm_ffn"] = gain((E, D_MODEL))
    inp["ffn_w_gate"] = nrm((E, D_MODEL, D_FF), D_MODEL ** -0.5)
    inp["ffn_w_up"] = nrm((E, D_MODEL, D_FF), D_MODEL ** -0.5)
    inp["ffn_w_down"] = nrm((E, D_FF, D_MODEL), D_FF ** -0.5)
    inp["od_norm_mix"] = gain((O, D_MODEL))
    inp["od_w_in"] = nrm((O, D_MODEL, ODD_IN), D_MODEL ** -0.5)
    inp["fox_b_f"] = jax.random.uniform(next(ks), (O, FOX_HEADS), F32, 1.0, 6.0)
    inp["od_w_out"] = nrm((O, FOX_WIDTH, D_MODEL), FOX_WIDTH ** -0.5)
    inp["od_norm_ffn"] = gain((O, D_MODEL))
    inp["moe_w_router"] = nrm((O, D_MODEL, N_EXPERTS), D_MODEL ** -0.5)
    inp["moe_w_gate"] = nrm((O, N_EXPERTS, D_MODEL, D_FF_EXPERT), D_MODEL ** -0.5)
    inp["moe_w_up"] = nrm((O, N_EXPERTS, D_MODEL, D_FF_EXPERT), D_MODEL ** -0.5)
    inp["moe_w_down"] = nrm((O, N_EXPERTS, D_FF_EXPERT, D_MODEL), D_FF_EXPERT ** -0.5)
    inp["final_norm"] = gain((D_MODEL,))
    return inp


def reference(x, ev_norm_mix, ev_w_in, s5_a_re, s5_a_im, s5_b_re, s5_b_im, s5_c_re, s5_c_im,
              s5_d, s5_log_step, s5_w_glu, s5_b_glu, ev_w_out, ev_norm_ffn, ffn_w_gate, ffn_w_up,
              ffn_w_down, od_norm_mix, od_w_in, fox_b_f, od_w_out, od_norm_ffn, moe_w_router,
              moe_w_gate, moe_w_up, moe_w_down, final_norm):
    for layer in range(DEPTH):
        i = layer // 2
        if layer % 2 == 0:
            h = rmsnorm(x, ev_norm_mix[i])
            proj = h @ ev_w_in[i]
            u, q, k, v = jnp.split(proj, [S5_WIDTH, S5_WIDTH + SB_WIDTH, S5_WIDTH + 2 * SB_WIDTH], axis=-1)
            a_out = s5_mixer(u, s5_a_re[i], s5_a_im[i], s5_b_re[i], s5_b_im[i], s5_c_re[i],
                             s5_c_im[i], s5_d[i], s5_log_step[i], s5_w_glu[i], s5_b_glu[i])
            b_out = from_heads(stick_breaking_attention(to_heads(q, SB_HEADS, SB_HEAD_DIM),
                                                        to_heads(k, SB_HEADS, SB_HEAD_DIM),
                                                        to_heads(v, SB_HEADS, SB_HEAD_DIM)))
            x = x + jnp.concatenate([a_out, b_out], axis=-1) @ ev_w_out[i]
            h = rmsnorm(x, ev_norm_ffn[i])
            x = x + swiglu(h, ffn_w_gate[i], ffn_w_up[i], ffn_w_down[i])
        else:
            h = rmsnorm(x, od_norm_mix[i])
            proj = h @ od_w_in[i]
            q, k, v, f_logit = jnp.split(proj, [FOX_WIDTH, 2 * FOX_WIDTH, 3 * FOX_WIDTH], axis=-1)
            log_f = jax.nn.log_sigmoid(f_logit.astype(F32) + fox_b_f[i].astype(F32))
            c_out = from_heads(forgetting_attention(to_heads(q, FOX_HEADS, FOX_HEAD_DIM),
                                                    to_heads(k, FOX_HEADS, FOX_HEAD_DIM),
                                                    to_heads(v, FOX_HEADS, FOX_HEAD_DIM),
                                                    log_f.transpose(0, 2, 1)))
            x = x + c_out @ od_w_out[i]
            h = rmsnorm(x, od_norm_ffn[i])
            x = x + moe_swiglu(h, moe_w_router[i], moe_w_gate[i], moe_w_up[i], moe_w_down[i])
    return rmsnorm(x, final_norm)
```

```python
import numpy as np
from contextlib import ExitStack
import ml_dtypes
import concourse.bass as bass
import concourse.mybir as mybir
from concourse.bass_utils import run_bass_kernel_spmd

F32 = mybir.dt.float32
BF16 = mybir.dt.bfloat16
AF = mybir.ActivationFunctionType
ALU = mybir.AluOpType
AX = mybir.AxisListType
NPBF = ml_dtypes.bfloat16

NCORES = 8
D = 1024
B = 2
L = 8192
TPC = B * L // NCORES
EPS = 1e-6
EVAC_MODE = 'mix'
SB_SOFTPLUS = False


class PairView:
    def __init__(self, fn):
        self.fn = fn

    def __getitem__(self, key):
        return self.fn(key[0])[tuple(key[1:])]


class Prog:
    ENGS = ("pe", "act", "dve", "pool", "sp")

    def __init__(self):
        self.nc = bass.Bass("TRN2", target_bir_lowering=False)
        self.sem_es = ExitStack()
        self.es = ExitStack()
        self.ops = {e: [] for e in self.ENGS}
        self.sems = {}
        self.handles = {}
        self.val = {}
        self.nfree = {}
        self.seen = {e: {} for e in self.ENGS}
        self.st = {}
        self.phase = 0
        self.barrier = {}
        self.io = {}

    def sem(self, key, q="sp"):
        if key in self.ENGS:
            phys = key
        else:
            if key not in self.sems:
                n = self.nfree.get(q, 0)
                self.sems[key] = f"d{q}{n}"
                self.nfree[q] = n + 1
            phys = self.sems[key]
        if phys not in self.handles:
            self.handles[phys] = self.sem_es.enter_context(self.nc.semaphore("s_" + phys))
            self.val[phys] = 0
        return phys

    def sb(self, name, shape, dt):
        return self.es.enter_context(self.nc.sbuf_tensor(f"p{self.phase}_{name}", list(shape), dt))

    def ps(self, name, shape, dt=F32):
        return self.es.enter_context(self.nc.psum_tensor(f"p{self.phase}_{name}", list(shape), dt))

    def dram(self, name, shape, dt, kind):
        return self.nc.dram_tensor(name, list(shape), dt, kind=kind)

    def D(self, name, shape, dt, kind):
        if name in self.io:
            return self.io[name]
        return self.dram(name, shape, dt, kind).ap()

    def _deps(self, eng, reads, writes):
        need = {}

        def add(sv):
            if sv is None:
                return
            k, v = sv
            if need.get(k, 0) < v:
                need[k] = v

        for r in reads:
            s = self.st.get(r)
            if s:
                add(s[0])
        for w in writes:
            s = self.st.get(w)
            if s:
                add(s[0])
                for k, v in s[1].items():
                    add((k, v))
        waits = []
        for k, v in need.items():
            if eng == "pe" and k == "pe":
                continue
            if self.seen[eng].get(k, 0) >= v:
                continue
            self.seen[eng][k] = v
            waits.append((k, v))
        return waits

    def _commit(self, semkey, val, reads, writes):
        for r in reads:
            s = self.st.setdefault(r, [None, {}])
            s[1][semkey] = val
        for w in writes:
            self.st[w] = [(semkey, val), {}]

    LIMIT = None
    NOPS = 0

    def op(self, eng, fn, reads=(), writes=()):
        Prog.NOPS += 1
        if Prog.LIMIT is not None and Prog.NOPS > Prog.LIMIT:
            return
        waits = self._deps(eng, reads, writes)
        self.sem(eng)
        self.val[eng] += 1
        v = self.val[eng]
        self._commit(eng, v, reads, writes)
        self.ops[eng].append((waits, fn, eng, 1))

    def dma(self, q, out, in_, semkey, reads=(), writes=(), **kw):
        Prog.NOPS += 1
        if Prog.LIMIT is not None and Prog.NOPS > Prog.LIMIT:
            return
        waits = self._deps(q, reads, writes)
        sk = self.sem(semkey, q)
        self.val[sk] += 16
        v = self.val[sk]
        self._commit(sk, v, reads, writes)
        self.ops[q].append((waits, lambda e: e.dma_start(out=out, in_=in_, **kw), sk, 16))

    def phase_end(self, final=False):
        nc = self.nc
        barrier = [(k, v) for k, v in self.barrier.items() if v > 0]
        fin = [(k, v) for k, v in self.val.items() if v > 0] if final else []

        def run(name):
            def f(e):
                for k, v in barrier:
                    e.wait_ge(self.handles[k], v)
                for waits, fn, sk, inc in self.ops[name]:
                    for k, v in waits:
                        e.wait_ge(self.handles[k], v)
                    fn(e).then_inc(self.handles[sk], inc)
                if name == "sp":
                    for k, v in fin:
                        e.wait_ge(self.handles[k], v)
            return f

        with nc.Block() as block:
            block.tensor(run("pe"))
            block.scalar(run("act"))
            block.vector(run("dve"))
            block.gpsimd(run("pool"))
            block.sync(run("sp"))
        self.es.close()
        self.es = ExitStack()
        self.ops = {e: [] for e in self.ENGS}
        self.barrier = dict(self.val)
        self.seen = {e: dict(self.val) for e in self.ENGS}
        self.st = {}
        self.sems = {}
        self.nfree = {}
        self.phase += 1

    def build(self):
        self.phase_end(final=True)
        self.sem_es.close()
        return self.nc


def ident_bf16(p, name="ident"):
    idf = p.sb(name + "_f", [128, 128], F32)
    idb = p.sb(name, [128, 128], BF16)
    p.op("pool", lambda e: e.memset(idf[:], 1.0), writes=[name + "_f"])
    p.op("pool", lambda e: e.affine_select(out=idf[:], in_=idf[:], pattern=[[1, 128]],
                                           compare_op=ALU.is_equal, fill=0.0, base=0,
                                           channel_multiplier=-1),
         reads=[name + "_f"], writes=[name + "_f"])
    p.op("dve", lambda e: e.tensor_copy(out=idb[:], in_=idf[:]), reads=[name + "_f"], writes=[name])
    return idb


def norm_scratch(p, tag="N", with_xt=True):
    sc = {}
    sc["xt"] = [p.sb(f"{tag}_xt{i}", [128, D], F32) for i in range(2)] if with_xt else None
    sc["junk"] = p.sb(f"{tag}_junk", [128, D], F32)
    sc["hn"] = [p.sb(f"{tag}_hn{i}", [128, D], BF16) for i in range(2)]
    sc["ss"] = [p.sb(f"{tag}_ss{i}", [128, 1], F32) for i in range(2)]
    sc["rs"] = [p.sb(f"{tag}_rs{i}", [128, 1], F32) for i in range(2)]
    sc["pst"] = [p.ps(f"{tag}_pst{i}", [128, D], BF16) for i in range(2)]
    sc["epsb"] = p.sb(f"{tag}_epsb", [128, 1], F32)
    sc["tag"] = tag
    p.op("pool", lambda e: e.memset(sc["epsb"][:], EPS), writes=["epsb"])
    return sc


def emit_norm_transpose(p, x_dram, gainT, gkey, hT, hkey, ident, ntiles, sc, xres=None, tiles=None):
    tag = sc["tag"]
    xt, junk, hn, ss, rs, pst, epsb = sc["xt"], sc["junk"], sc["hn"], sc["ss"], sc["rs"], sc["pst"], sc["epsb"]
    for i in (range(ntiles) if tiles is None else tiles):
        b = i % 2
        if xres is None:
            p.dma("sp", xt[b][:], x_dram[i * 128:(i + 1) * 128, :], f"{tag}_ld{b}", writes=[f"{tag}_xt{b}"])
            srcap = xt[b][:]
            rk = f"{tag}_xt{b}"
        else:
            srcap = xres[:, i, :]
            rk = f"xres{i}"
        p.op("act", lambda e, srcap=srcap, b=b: e.activation(out=junk[:], in_=srcap, func=AF.Square, accum_out=ss[b][:]),
             reads=[rk], writes=[f"{tag}_junk", f"{tag}_ss{b}"])
        p.op("act", lambda e, b=b: e.activation(out=rs[b][:], in_=ss[b][:], func=AF.Sqrt, scale=1.0 / D, bias=epsb[:, 0:1]),
             reads=[f"{tag}_ss{b}", "epsb"], writes=[f"{tag}_rs{b}"])
        p.op("dve", lambda e, b=b: e.reciprocal(out=rs[b][:], in_=rs[b][:]), reads=[f"{tag}_rs{b}"], writes=[f"{tag}_rs{b}"])
        p.op("dve", lambda e, srcap=srcap, b=b: e.tensor_scalar(out=hn[b][:], in0=srcap, scalar1=rs[b][:, 0:1], scalar2=None, op0=ALU.mult),
             reads=[rk, f"{tag}_rs{b}"], writes=[f"{tag}_hn{b}"])
        for c in range(8):
            p.op("pe", lambda e, b=b, c=c: e.transpose(out=pst[b][:, c * 128:(c + 1) * 128], in_=hn[b][:, c * 128:(c + 1) * 128], identity=ident[:]),
                 reads=[f"{tag}_hn{b}", "ident"], writes=[f"{tag}_pst{b}"])
        for c in range(8):
            eng = "act" if b == 0 else "dve"
            if eng == "act":
                fn = lambda e, b=b, c=c, i=i: e.activation(out=hT[:, c, i * 128:(i + 1) * 128], in_=pst[b][:, c * 128:(c + 1) * 128],
                                                           func=AF.Copy, scale=gainT[:, c:c + 1])
            else:
                fn = lambda e, b=b, c=c, i=i: e.tensor_scalar(out=hT[:, c, i * 128:(i + 1) * 128], in0=pst[b][:, c * 128:(c + 1) * 128],
                                                              scalar1=gainT[:, c:c + 1], scalar2=None, op0=ALU.mult)
            p.op(eng, fn, reads=[f"{tag}_pst{b}", gkey], writes=[f"{hkey}_{c}_{i // 4}"])


def build_phase_a(nsub=1, p=None):
    own = p is None
    if own:
        p = Prog()
    nc = p.nc
    x = p.D("x", [nsub * TPC, D], F32, "ExternalInput")
    gain = p.D("gain", [128, 8], F32, "ExternalInput")
    w = p.D("w", [D, 2048], F32, "ExternalInput")
    featT = p.D("featT", [1536, nsub * TPC], BF16, "ExternalOutput")
    vout = p.D("v", [nsub * TPC, 512], BF16, "ExternalOutput")
    NT = TPC // 128
    ident = ident_bf16(p)
    gainT = p.sb("gainT", [128, 8], F32)
    p.dma("sp", gainT[:], gain[:, :], "ld_gain", writes=["A_gain"])
    sc = norm_scratch(p)
    wsb = p.sb("wsb", [128, 8, 2048], BF16)
    wv = w.rearrange("(c p) n -> p c n", p=128)
    for c in range(8):
        p.dma("pool", wsb[:, c, :], wv[:, c, :], f"ld_w{c}", writes=[f"w{c}"])
    hTs = [p.sb(f"hT{j}", [128, 8, TPC], BF16) for j in range(2 if nsub > 1 else 1)]
    ob = [p.sb(f"ob{i}", [128, 512], BF16) for i in range(3)]
    pm = [p.ps(f"pm{i}", [128, 512], F32) for i in range(4)]
    k = 0

    def norm_tiles(sub, i0, i1):
        j = sub % len(hTs)
        emit_norm_transpose(p, x[sub * TPC:(sub + 1) * TPC, :], gainT, "A_gain", hTs[j], f"A_hT{j}", ident, NT, sc, tiles=range(i0, i1))

    norm_tiles(0, 0, NT)
    for sub in range(nsub):
        hT = hTs[sub % len(hTs)]
        hk = f"A_hT{sub % len(hTs)}"
        nxt = 0
        for n in range(12):
            if sub + 1 < nsub and n > 0:
                cnt = 2 if n <= 5 else 1
                norm_tiles(sub + 1, nxt, min(NT, nxt + cnt))
                nxt = min(NT, nxt + cnt)
            for t in range(TPC // 512):
                pb = k % 4
                sbi = k % 3
                for c in range(8):
                    p.op("pe", lambda e, pb=pb, c=c, n=n, t=t, hT=hT: e.matmul(pm[pb][:], lhsT=wsb[:, c, n * 128:(n + 1) * 128],
                                                                        rhs=hT[:, c, t * 512:(t + 1) * 512],
                                                                        start=(c == 0), stop=(c == 7)),
                         reads=[f"w{c}", f"{hk}_{c}_{t}"], writes=[f"pm{pb}"])
                qs_ = 0.125 if 4 <= n < 8 else 1.0
                if k % 2 == 0:
                    p.op("act", lambda e, pb=pb, sbi=sbi, qs_=qs_: e.activation(out=ob[sbi][:], in_=pm[pb][:], func=AF.Copy, scale=qs_),
                         reads=[f"pm{pb}"], writes=[f"ob{sbi}"])
                else:
                    p.op("dve", lambda e, pb=pb, sbi=sbi, qs_=qs_: e.tensor_scalar(out=ob[sbi][:], in0=pm[pb][:], scalar1=qs_, scalar2=None, op0=ALU.mult),
                         reads=[f"pm{pb}"], writes=[f"ob{sbi}"])
                p.dma("sp", featT[n * 128:(n + 1) * 128, sub * TPC + t * 512:sub * TPC + (t + 1) * 512], ob[sbi][:], f"st_ob{sbi}", reads=[f"ob{sbi}"])
                k += 1
        if sub + 1 < nsub and nxt < NT:
            norm_tiles(sub + 1, nxt, NT)
        for i in range(NT):
            pb = k % 4
            sbi = k % 3
            for c in range(8):
                p.op("pe", lambda e, pb=pb, c=c, i=i, hT=hT: e.matmul(pm[pb][:], lhsT=hT[:, c, i * 128:(i + 1) * 128],
                                                               rhs=wsb[:, c, 1536:2048], start=(c == 0), stop=(c == 7)),
                     reads=[f"w{c}", f"{hk}_{c}_{i // 4}"], writes=[f"pm{pb}"])
            if k % 2 == 0:
                p.op("act", lambda e, pb=pb, sbi=sbi: e.activation(out=ob[sbi][:], in_=pm[pb][:], func=AF.Copy),
                     reads=[f"pm{pb}"], writes=[f"ob{sbi}"])
            else:
                p.op("dve", lambda e, pb=pb, sbi=sbi: e.tensor_copy(out=ob[sbi][:], in_=pm[pb][:]),
                     reads=[f"pm{pb}"], writes=[f"ob{sbi}"])
            p.dma("sp", vout[sub * TPC + i * 128:sub * TPC + (i + 1) * 128, :], ob[sbi][:], f"st_ob{sbi}", reads=[f"ob{sbi}"])
            k += 1
    return p.build() if own else p.phase_end()


def build_attn(mode, npairs, LL=L, qcs=None, p=None, side=None, side_every=4, n_ps_s=3):
    own = p is None
    if own:
        p = Prog()
    fox = mode == "fox"
    NKB = LL // 128
    NQC = LL // 512
    KA = 69 if fox else 64
    QCS = list(qcs) if qcs is not None else list(range(LL // 512))
    VW = 65 if fox else 64
    qT = p.D("qT", [npairs, 64, LL], BF16, "ExternalInput")
    kT = p.D("kT", [npairs, 64, LL], BF16, "ExternalInput")
    v = p.D("v", [npairs, LL, 64], BF16, "ExternalInput")
    oT = p.D("oT", [npairs, 64, len(QCS) * 512], BF16, "ExternalOutput")
    qa = [p.sb(f"qa{i}", [KA, LL], BF16) for i in range(2)]
    ka = [p.sb(f"ka{i}", [KA, LL], BF16) for i in range(2)]
    va = [p.sb(f"va{i}", [128, NKB, VW], BF16) for i in range(2)]
    NPT = 4
    pt = [p.sb(f"pt{i}", [128, 512], BF16) for i in range(NPT)]
    ptd = [p.sb(f"ptd{j}", [128, 512], BF16) for j in range(4)]
    for j in range(4):
        p.op("pool", lambda e, j=j: e.memset(ptd[j][:], 0.0), writes=[f"ptd{j}"])
    if not fox:
        n_ps_s = 5
    ps_s = [p.ps(f"ps_s{i}", [128, 512], F32) for i in range(n_ps_s)]
    ps_o = [p.ps(f"ps_o{i}", [128, 512], F32) for i in range(2)]
    osb = p.sb("osb", [VW, 512], F32)
    ob = [p.sb(f"ob{i}", [64, 512], BF16) for i in range(2)]
    if fox:
        ps_b = p.ps("ps_b", [128, 512], F32)
        rinv = p.sb("rinv", [VW, 512], F32)
        onesf = p.sb("onesf", [VW, 64], F32)
        p.op("pool", lambda e: e.memset(onesf[:], 1.0), writes=["onesf"])
        SEG = LL * npairs // 128
        SPP = 128 // npairs
        fl_d = p.D("fl", [128, SEG], F32, "ExternalInput")
        padrow = p.D("padrow", [1, LL], BF16, "ExternalInput")
        nb_d = p.D("negb", [128, 1], F32, "ExternalInput")
        fl = p.sb("fl_sb", [128, SEG], F32)
        cn = p.sb("cn", [128, SEG], F32)
        nb = p.sb("nb", [128, 1], F32)
        tot = p.sb("tot", [128, 1], F32)
        off = p.sb("off", [128, 1], F32)
        tri = p.sb("tri", [128, 128], F32)
        hi = p.sb("hi", [128, SEG], BF16)
        nhi = p.sb("nhi", [128, SEG], BF16)
        mid = p.sb("mid", [128, SEG], BF16)
        lo = p.sb("lo", [128, SEG], BF16)
        ps_c = p.ps("ps_c", [128, 1], F32)
        p.dma("sp", fl[:], fl_d[:, :], "ld_fl", writes=["fl"])
        p.dma("sp", nb[:], nb_d[:, :], "ld_nb", writes=["nb"])
        p.op("pool", lambda e: e.memset(tri[:], 1.0), writes=["tri"])
        p.op("pool", lambda e: e.affine_select(out=tri[:], in_=tri[:], pattern=[[1, 128]], compare_op=ALU.is_gt,
                                               fill=0.0, base=0, channel_multiplier=-1), reads=["tri"], writes=["tri"])
        for a in range(1, npairs):
            p.op("pool", lambda e, a=a: e.affine_select(out=tri[:, a * SPP:(a + 1) * SPP], in_=tri[:, a * SPP:(a + 1) * SPP],
                                                        pattern=[[0, SPP]], compare_op=ALU.is_ge, fill=0.0,
                                                        base=-a * SPP, channel_multiplier=1), reads=["tri"], writes=["tri"])
        p.op("act", lambda e: e.activation(out=fl[:], in_=fl[:], func=AF.Exp, scale=-1.0, bias=nb[:, 0:1]),
             reads=["fl", "nb"], writes=["fl"])
        p.op("act", lambda e: e.activation(out=fl[:], in_=fl[:], func=AF.Ln, bias=onesf[:, 0:1] if False else 1.0),
             reads=["fl"], writes=["fl"])
        p.op("dve", lambda e: e.tensor_scalar(out=fl[:], in0=fl[:], scalar1=0.5, scalar2=None, op0=ALU.mult),
             reads=["fl"], writes=["fl"])
        p.op("dve", lambda e: e.tensor_tensor_scan(out=cn[:], data0=fl[:], data1=fl[:], initial=0.0,
                                                   op0=ALU.add, op1=ALU.add), reads=["fl"], writes=["cn"])
        p.op("dve", lambda e: e.tensor_copy(out=tot[:], in_=cn[:, SEG - 1:SEG]), reads=["cn"], writes=["tot"])
        p.op("pe", lambda e: e.matmul(ps_c[:], lhsT=tri[:], rhs=tot[:], start=True, stop=True),
             reads=["tri", "tot"], writes=["ps_c"])
        p.op("dve", lambda e: e.tensor_copy(out=off[:], in_=ps_c[:]), reads=["ps_c"], writes=["off"])
        p.op("dve", lambda e: e.tensor_scalar(out=cn[:], in0=cn[:], scalar1=off[:, 0:1], scalar2=None, op0=ALU.add),
             reads=["cn", "off"], writes=["cn"])
        p.op("dve", lambda e: e.tensor_copy(out=hi[:], in_=cn[:]), reads=["cn"], writes=["hi"])
        p.op("dve", lambda e: e.tensor_scalar(out=nhi[:], in0=hi[:], scalar1=-1.0, scalar2=None, op0=ALU.mult),
             reads=["hi"], writes=["nhi"])
        p.op("dve", lambda e: e.tensor_tensor(out=cn[:], in0=cn[:], in1=hi[:], op=ALU.subtract), reads=["cn", "hi"], writes=["cn"])
        p.op("dve", lambda e: e.tensor_copy(out=mid[:], in_=cn[:]), reads=["cn"], writes=["mid"])
        p.op("dve", lambda e: e.tensor_tensor(out=cn[:], in0=cn[:], in1=mid[:], op=ALU.subtract), reads=["cn", "mid"], writes=["cn"])
        p.op("dve", lambda e: e.tensor_copy(out=lo[:], in_=cn[:]), reads=["cn"], writes=["lo"])
        cx = p.dram("cx", [4, 128 * SEG], BF16, "Internal").ap()
        cxf = cx
        for r, src, nm in ((0, nhi, "nhi"), (1, hi, "hi"), (2, mid, "mid"), (3, lo, "lo")):
            p.dma("sp", cx[r, :].rearrange("(q f) -> q f", f=SEG), src[:], f"st_cx{r}", reads=[nm], writes=[f"cx{r}"])
    else:
        ntri_f = p.sb("ntri_f", [128, 128], F32)
        ntri = p.sb("ntri", [128, 128], BF16)
        nones = p.sb("nones", [128, 128], BF16)
        p.op("pool", lambda e: e.memset(ntri_f[:], -1.0), writes=["ntri_f"])
        p.op("pool", lambda e: e.affine_select(out=ntri_f[:], in_=ntri_f[:], pattern=[[-1, 128]], compare_op=ALU.is_ge,
                                               fill=0.0, base=0, channel_multiplier=1), reads=["ntri_f"], writes=["ntri_f"])
        p.op("dve", lambda e: e.tensor_copy(out=ntri[:], in_=ntri_f[:]), reads=["ntri_f"], writes=["ntri"])
        p.op("pool", lambda e: e.memset(nones[:], -1.0), writes=["nones"])
        LSr = [p.sb(f"LS{i}", [128, 512], BF16) for i in range(2)]
        lb = [p.sb(f"lb{i}", [128, 512], BF16) for i in range(4)]
        et = [p.sb(f"et{i}", [128, 512], F32) for i in range(2)]

    def load_pair(pi):
        if pi >= npairs:
            return
        b = pi % 2
        p.dma("sp", qa[b][0:64, :], qT[pi, :, :], f"ld_q{b}", writes=[f"qa{b}"])
        p.dma("sp", ka[b][0:64, :], kT[pi, :, :], f"ld_k{b}", writes=[f"ka{b}"])
        p.dma("sp", va[b][:, :, 0:64], v[pi, :, :].rearrange("(kb p) d -> p kb d", p=128), f"ld_v{b}", writes=[f"va{b}"])
        if fox:
            p.op("pool", lambda e, b=b: e.memset(va[b][:, :, 64:65], 1.0), reads=[], writes=[f"va1_{b}"])
            p.op("pool", lambda e, b=b: e.memset(qa[b][64:69, :], 1.0), writes=[f"qax{b}"])
            p.op("pool", lambda e, b=b: e.memset(ka[b][64:69, :], 1.0), writes=[f"kax{b}"])
            p.dma("sp", qa[b][64:65, :], cxf[0:1, pi * LL:(pi + 1) * LL], f"ld_qx{b}", reads=["cx0"], writes=[f"qax{b}"])
            for r in (65, 66, 67):
                p.dma("sp", ka[b][r:r + 1, :], cxf[r - 64:r - 63, pi * LL:(pi + 1) * LL], f"ld_kx{b}_{r}", reads=[f"cx{r - 64}"], writes=[f"kax{b}"])
            p.dma("sp", ka[b][68:69, :], padrow[0:1, :], f"ld_kx{b}_68", writes=[f"kax{b}"])

    tiles = []
    for pi in range(npairs):
        for qi_, qc in enumerate(QCS):
            nkb = 4 * qc + 4
            order = list(range(nkb)) if fox else list(range(nkb - 1, -1, -1))
            for n_i, kb in enumerate(order):
                j = kb - 4 * qc
                diag = j >= 0
                c0 = 128 * j if diag else 0
                tiles.append(dict(pi=pi, b=pi % 2, qc=qc, kb=kb, n_i=n_i, nkb=nkb, j=j, diag=diag, c0=c0, gid=pi * len(QCS) + qi_, qi=qi_,
                                  last_of_pair=(qi_ == len(QCS) - 1 and n_i == nkb - 1)))
    NTILES = len(tiles)
    ring = {"pt": 0, "lb": 0}
    deferred = {}

    def keys_of(t):
        b = t["b"]
        qk = [f"qa{b}"] + ([f"qax{b}"] if fox else [])
        kk = [f"ka{b}"] + ([f"kax{b}"] if fox else [])
        vk = [f"va{b}"] + ([f"va1_{b}"] if fox else [])
        return qk, kk, vk

    def slices_of(t):
        cs = slice(t["c0"], 512)
        qs = slice(t["qc"] * 512 + t["c0"], (t["qc"] + 1) * 512)
        ks = slice(t["kb"] * 128, (t["kb"] + 1) * 128)
        return cs, qs, ks

    def alloc_P(t):
        if t["diag"]:
            t["P"], t["pk"] = ptd[t["j"]], f"ptd{t['j']}"
        else:
            t["P"], t["pk"] = pt[ring["pt"] % NPT], f"pt{ring['pt'] % NPT}"
            ring["pt"] += 1

    def mask_P(t):
        if t["diag"]:
            P, pk, c0 = t["P"], t["pk"], t["c0"]
            p.op("pool", lambda e, P=P, c0=c0: e.affine_select(out=P[:, c0:c0 + 128], in_=P[:, c0:c0 + 128], pattern=[[1, 128]],
                                                              compare_op=(ALU.is_ge if fox else ALU.is_gt), fill=0.0, base=0, channel_multiplier=-1),
                 reads=[pk], writes=[pk])

    def stage_A(i, t):
        b = t["b"]
        qk, kk, vk = keys_of(t)
        cs, qs, ks = slices_of(t)
        si = i % n_ps_s
        pss = ps_s[si]
        t["pss"], t["psk"] = pss, f"ps_s{si}"
        p.op("pe", lambda e, pss=pss, cs=cs, ks=ks, qs=qs, b=b: e.matmul(pss[:, cs], lhsT=ka[b][:, ks], rhs=qa[b][:, qs], start=True, stop=True),
             reads=qk + kk, writes=[f"ps_s{si}"])
        if fox:
            alloc_P(t)
            P, pk = t["P"], t["pk"]
            p.op("act", lambda e, P=P, pss=pss, cs=cs: e.activation(out=P[:, cs], in_=pss[:, cs], func=AF.Exp), reads=[f"ps_s{si}"], writes=[pk])
            mask_P(t)
        else:
            li = ring["lb"] % 4
            ring["lb"] += 1
            ei = i % 2
            L_, lk = lb[li], f"lb{li}"
            E_, ek = et[ei], f"et{ei}"
            t["L"], t["lk"] = L_, lk
            if SB_SOFTPLUS:
                p.op("act", lambda e, L_=L_, pss=pss, cs=cs: e.activation(out=L_[:, cs], in_=pss[:, cs], func=AF.Softplus), reads=[f"ps_s{si}"], writes=[lk])
            else:
                p.op("act", lambda e, E_=E_, pss=pss, cs=cs: e.activation(out=E_[:, cs], in_=pss[:, cs], func=AF.Exp), reads=[f"ps_s{si}"], writes=[ek])
                p.op("act", lambda e, E_=E_, L_=L_, cs=cs: e.activation(out=L_[:, cs], in_=E_[:, cs], func=AF.Ln, bias=1.0), reads=[ek], writes=[lk])
            if t["diag"]:
                c0 = t["c0"]
                p.op("pool", lambda e, L_=L_, c0=c0: e.affine_select(out=L_[:, c0:c0 + 128], in_=L_[:, c0:c0 + 128], pattern=[[1, 128]],
                                                                    compare_op=ALU.is_gt, fill=0.0, base=0, channel_multiplier=-1), reads=[lk], writes=[lk])

    def stage_B_sb(i, t):
        cs, qs, ks = slices_of(t)
        L_, lk = t["L"], t["lk"]
        pa, pak = t["pss"], t["psk"]
        alloc_P(t)
        P, pk = t["P"], t["pk"]
        first = t["n_i"] == 0
        p.op("pe", lambda e, pa=pa, cs=cs, L_=L_, first=first: e.matmul(pa[:, cs], lhsT=ntri[:], rhs=L_[:, cs], start=False, stop=first, skip_group_check=True), reads=["ntri", lk], writes=[pak])
        li_ = t["n_i"] % 2
        LS, LSk = LSr[li_], f"LS{li_}"
        LSn, LSnk = LSr[1 - li_], f"LS{1 - li_}"
        if not first:
            p.op("pe", lambda e, pa=pa, cs=cs, LS=LS: e.matmul(pa[:, cs], lhsT=nones[:], rhs=LS[:, cs], start=False, stop=True, skip_group_check=True),
                 reads=["nones", LSk], writes=[pak])
        p.op("act", lambda e, P=P, pa=pa, cs=cs: e.activation(out=P[:, cs], in_=pa[:, cs], func=AF.Exp), reads=[pak], writes=[pk])
        mask_P(t)
        if t["n_i"] + 1 < t["nkb"]:
            c0 = t["c0"]
            if c0 > 0:
                p.op("pool", lambda e, LSn=LSn, c0=c0: e.memset(LSn[:, 0:c0], 0.0), writes=[LSnk])
            if first:
                p.op("pool", lambda e, LSn=LSn, L_=L_, cs=cs: e.tensor_copy(out=LSn[:, cs], in_=L_[:, cs]), reads=[lk, LSnk], writes=[LSnk])
            else:
                p.op("pool", lambda e, LSn=LSn, LS=LS, L_=L_, cs=cs: e.tensor_tensor(out=LSn[:, cs], in0=LS[:, cs], in1=L_[:, cs], op=ALU.add),
                     reads=[lk, LSk, LSnk], writes=[LSnk])

    def finalize_1(t):
        po, pok = ps_o[t["gid"] % 2], f"ps_o{t['gid'] % 2}"
        obi = t["gid"] % 2
        if fox:
            p.op("act", lambda e, po=po: e.activation(out=osb[:], in_=po[0:VW, :], func=AF.Copy), reads=[pok], writes=["osb"])
            p.op("dve", lambda e: e.reciprocal(out=rinv[64:65, :], in_=osb[64:65, :]), reads=["osb"], writes=["rinv"])
        else:
            p.op("act", lambda e, po=po, obi=obi: e.activation(out=ob[obi][:], in_=po[0:64, :], func=AF.Copy), reads=[pok], writes=[f"ob{obi}"])
            p.dma("sp", oT[t["pi"], :, t["qi"] * 512:(t["qi"] + 1) * 512], ob[obi][:], f"st_o{obi}", reads=[f"ob{obi}"])

    def finalize_2(t):
        obi = t["gid"] % 2
        p.op("pe", lambda e: e.matmul(ps_b[0:64, :], lhsT=onesf[64:65, :], rhs=rinv[64:65, :], start=True, stop=True), reads=["onesf", "rinv"], writes=["ps_b"])
        p.op("dve", lambda e, obi=obi: e.tensor_tensor(out=ob[obi][:], in0=osb[0:64, :], in1=ps_b[0:64, :], op=ALU.mult), reads=["osb", "ps_b"], writes=[f"ob{obi}"])
        p.dma("sp", oT[t["pi"], :, t["qi"] * 512:(t["qi"] + 1) * 512], ob[obi][:], f"st_o{obi}", reads=[f"ob{obi}"])

    def stage_PV(i, t):
        b = t["b"]
        qk, kk, vk = keys_of(t)
        po, pok = ps_o[t["gid"] % 2], f"ps_o{t['gid'] % 2}"
        P, pk, kb, n_i, nkb = t["P"], t["pk"], t["kb"], t["n_i"], t["nkb"]
        p.op("pe", lambda e, po=po, P=P, kb=kb, b=b, n_i=n_i, nkb=nkb: e.matmul(po[0:VW, :], lhsT=va[b][:, kb, :], rhs=P[:, :], start=(n_i == 0), stop=(n_i == nkb - 1)),
             reads=vk + [pk], writes=[pok])
        if n_i == nkb - 1:
            finalize_1(t)
            if fox:
                deferred.setdefault(i + 2, []).append(lambda t=t: finalize_2(t))
        if t["last_of_pair"]:
            load_pair(t["pi"] + 2)

    SK1 = 2
    SK2 = 2 if fox else 4
    rr = [0]
    load_pair(0)
    load_pair(1)
    for i in range(NTILES + SK2 + 3):
        if i < NTILES:
            stage_A(i, tiles[i])
        if not fox and 0 <= i - SK1 < NTILES:
            stage_B_sb(i, tiles[i - SK1])
        if 0 <= i - SK2 < NTILES:
            stage_PV(i, tiles[i - SK2])
        for fn in deferred.pop(i, []):
            fn()
        if side and (i % side_every == 0 if isinstance(side_every, int) else (i % side_every[0]) in side_every[1]):
            g = side[rr[0] % len(side)]
            rr[0] += 1
            try:
                next(g)
            except StopIteration:
                side.remove(g)
    assert not deferred
    while side:
        for g in list(side):
            try:
                next(g)
            except StopIteration:
                side.remove(g)
    return p.build() if own else p.phase_end()


PI = float(np.pi)


def ident_f32(p, name="identf"):
    idf = p.sb(name, [128, 128], F32)
    p.op("pool", lambda e: e.memset(idf[:], 1.0), writes=[name])
    p.op("pool", lambda e: e.affine_select(out=idf[:], in_=idf[:], pattern=[[1, 128]], compare_op=ALU.is_equal,
                                           fill=0.0, base=0, channel_multiplier=-1), reads=[name], writes=[name])
    return idf


def build_s5(nunits, LL=L, TC=2048, p=None, psx=None, defer=False):
    own = p is None
    if own:
        p = Prog()
    NK = int(np.log2(TC))
    NCH = LL // TC
    uT = p.D("uT", [nunits, 32, LL], BF16, "ExternalInput")
    prm = p.D("prm", [nunits, 128, 67], F32, "ExternalInput")
    dd = p.D("dd", [nunits, 32, 1], F32, "ExternalInput")
    yT = p.D("yT", [nunits, 32, LL], F32, "ExternalOutput")
    identf = ident_f32(p)
    pow2 = p.sb("pow2", [128, NK], F32)
    for k in range(NK):
        p.op("pool", lambda e, k=k: e.memset(pow2[:, k:k + 1], float(2 ** k)), writes=["pow2"])
    NS = 2
    ub = [p.sb(f"ub{i}", [32, LL], BF16) for i in range(NS)]
    P_ = [p.sb(f"prm{i}", [128, 67], F32) for i in range(NS)]
    dsb = [p.sb(f"dsb{i}", [32, 1], F32) for i in range(NS)]
    st_s = [{n: p.sb(f"{n}{i}", [128, TC], F32) for n in ("reA", "imA", "reB", "imB")} for i in range(NS)]
    rebf_s = [p.sb(f"rebf{i}", [128, TC], BF16) for i in range(NS)]
    rr_t_s = [p.sb(f"rr_t{i}", [128, NK], F32) for i in range(NS)]
    rr_i_s = [p.sb(f"rr_i{i}", [128, NK], mybir.dt.int32) for i in range(NS)]
    tmp2_s = [None] * NS
    dx_s = [p.sb(f"s5dx{i}", [32, 32], BF16) for i in range(NS)]
    imbf_s = [p.sb(f"imbf{i}", [128, TC], BF16) for i in range(NS)]
    p3bf_s = [p.sb(f"p3bf{i}", [128, TC], BF16) for i in range(NS)]
    p4bf_s = [p.sb(f"p4bf{i}", [128, TC], BF16) for i in range(NS)]
    zre_s = [p.sb(f"zre{i}", [128, TC], F32) for i in range(NS)]
    zim_s = [p.sb(f"zim{i}", [128, TC], F32) for i in range(NS)]
    cosT_s = [p.sb(f"cosT{i}", [128, TC], F32) for i in range(NS)]
    sinT_s = [p.sb(f"sinT{i}", [128, TC], F32) for i in range(NS)]
    rT_s = [p.sb(f"rT{i}", [128, TC], F32) for i in range(NS)]
    T1 = p.sb("s5T1", [128, TC], F32)
    rrT = p.sb("s5rrT", [128, TC], F32)
    rrI = p.sb("s5rrI", [128, TC], mybir.dt.int32)
    iota1 = p.sb("s5iota1", [128, TC], F32)
    assert TC == 512
    p.op("pool", lambda e: e.memset(T1[:], 0.5), writes=["T1"])
    p.op("dve", lambda e: e.tensor_tensor_scan(out=iota1[:], data0=T1[:], data1=T1[:], initial=0.0, op0=ALU.add, op1=ALU.add), reads=["T1"], writes=["iota1"])
    sm_s = [{n: p.sb(f"s5_{n}{i}", [128, NK], F32) for n in ("xk", "tk", "mag", "s1", "c1", "sin", "cos", "ar", "ai", "nai")} for i in range(NS)]
    col_s = [{n: p.sb(f"s5c_{n}{i}", [128, 1], F32) for n in ("dl", "x", "th", "m1", "den", "wre", "wim", "nwim", "t1", "t2", "cre", "cim", "t3", "t4")} for i in range(NS)]
    bb_s = [{n: p.sb(f"s5b_{n}{i}", [128, 16], F32) for n in ("bre", "bim")} for i in range(NS)]
    bx_s = [{n: p.sb(f"s5x_{n}{i}", [128, 32], F32) for n in ("bre", "bim")} for i in range(NS)]
    cxs_s = [{n: p.sb(f"s5cx_{n}{i}", [128, 32], BF16) for n in ("cre", "ncim", "ncre")} for i in range(NS)]
    lb_s = [{n: p.sb(f"s5l_{n}{i}", [32, 128], BF16) for n in ("bre", "bim")} for i in range(NS)]
    GLOBAL_KEYS = {"T1", "rrT", "rrI", "iota1", "pow2", "identf", "ps_t", "ps_bu0", "ps_bu1", "ps_bu2", "ps_bu3", "ps_y0", "ps_y1", "ysb0", "ysb1"}
    if psx is None:
        ps_t = p.ps("ps_t", [32, 128], F32)
        ps_bu = [p.ps(f"ps_bu{i}", [128, 512], F32) for i in range(4)]
        ps_y = [p.ps(f"ps_y{i}", [32, 512], F32) for i in range(2)]
    else:
        ps_t = psx[0:32, 0:128]
        ps_bu = [psx] * 4
        ps_y = [psx[0:32, :]] * 2
    ysb = [p.sb(f"ysb{i}", [32, 512], F32) for i in range(2)]

    def V(e, out, in0, s1, s2, op0, op1=None):
        if op1 is None:
            return e.tensor_scalar(out=out, in0=in0, scalar1=s1, scalar2=None, op0=op0)
        return e.tensor_scalar(out=out, in0=in0, scalar1=s1, scalar2=s2, op0=op0, op1=op1)

    iyc = [0]

    def unit_gen(u, b):
        def kk(k_):
            if psx is not None and k_.startswith("ps_"):
                return "psx"
            return k_ if k_ in GLOBAL_KEYS else f"s{b}_{k_}"

        def op(eng, fn, reads=(), writes=()):
            p.op(eng, fn, [kk(r) for r in reads], [kk(w) for w in writes])

        def dma(q, out, in_, semkey, reads=(), writes=(), **kw):
            p.dma(q, out, in_, semkey, [kk(r) for r in reads], [kk(w) for w in writes], **kw)

        st, rebf, imbf, tmp2, sm, col, bb, bx, cxs, lb_, rr_t, rr_i = (st_s[b], rebf_s[b], imbf_s[b], tmp2_s[b], sm_s[b], col_s[b], bb_s[b],
                                                                        bx_s[b], cxs_s[b], lb_s[b], rr_t_s[b], rr_i_s[b])
        dx = dx_s[b]
        p3bf, p4bf, zre, zim, cosT, sinT, rT = p3bf_s[b], p4bf_s[b], zre_s[b], zim_s[b], cosT_s[b], sinT_s[b], rT_s[b]
        dma("sp", ub[b][:], uT[u, :, :], f"ld_u{b}", writes=[f"ub{b}"])
        dma("sp", P_[b][:], prm[u, :, :], f"ld_p{b}", writes=[f"prm{b}"])
        dma("sp", dsb[b][:], dd[u, :, :], f"ld_d{b}", writes=[f"dsb{b}"])
        Pm = P_[b]
        pk = f"prm{b}"
        c = col
        op("act", lambda e, Pm=Pm: e.activation(out=c["dl"][:], in_=Pm[:, 2:3], func=AF.Exp), reads=[pk], writes=["c_dl"])
        op("dve", lambda e, Pm=Pm: e.tensor_tensor(out=c["x"][:], in0=Pm[:, 0:1], in1=c["dl"][:], op=ALU.mult), reads=[pk, "c_dl"], writes=["c_x"])
        op("dve", lambda e, Pm=Pm: e.tensor_tensor(out=c["th"][:], in0=Pm[:, 1:2], in1=c["dl"][:], op=ALU.mult), reads=[pk, "c_dl"], writes=["c_th"])
        op("dve", lambda e: V(e, sm["xk"][:], pow2[:], c["x"][:, 0:1], None, ALU.mult), reads=["pow2", "c_x"], writes=["xk"])
        op("dve", lambda e: V(e, sm["tk"][:], pow2[:], c["th"][:, 0:1], None, ALU.mult), reads=["pow2", "c_th"], writes=["tk"])
        op("act", lambda e: e.activation(out=sm["mag"][:], in_=sm["xk"][:], func=AF.Exp), reads=["xk"], writes=["mag"])
        for nm, shift in (("s1", 0.0), ("c1", 0.5 * PI)):
            R_ = sm[nm]
            op("dve", lambda e, R_=R_, shift=shift: V(e, R_[:], sm["tk"][:], shift, None, ALU.add), reads=["tk"], writes=[nm])
            op("dve", lambda e, R_=R_: V(e, rr_t[:], R_[:], 1.0 / (2 * PI), None, ALU.mult), reads=[nm], writes=["rr_t"])
            op("dve", lambda e: e.tensor_copy(out=rr_i[:], in_=rr_t[:]), reads=["rr_t"], writes=["rr_i"])
            op("dve", lambda e: e.tensor_copy(out=rr_t[:], in_=rr_i[:]), reads=["rr_i"], writes=["rr_t"])
            op("dve", lambda e, R_=R_: e.scalar_tensor_tensor(out=R_[:], in0=rr_t[:], scalar=-6.28125, in1=R_[:], op0=ALU.mult, op1=ALU.add),
                 reads=["rr_t", nm], writes=[nm])
            op("dve", lambda e, R_=R_: e.scalar_tensor_tensor(out=R_[:], in0=rr_t[:], scalar=-(2 * PI - 6.28125), in1=R_[:], op0=ALU.mult, op1=ALU.add),
                 reads=["rr_t", nm], writes=[nm])
            op("dve", lambda e, R_=R_: e.tensor_single_scalar(out=rr_t[:], in_=R_[:], scalar=PI, op=ALU.is_gt), reads=[nm], writes=["rr_t"])
            op("dve", lambda e, R_=R_: e.scalar_tensor_tensor(out=R_[:], in0=rr_t[:], scalar=-2 * PI, in1=R_[:], op0=ALU.mult, op1=ALU.add),
                 reads=["rr_t", nm], writes=[nm])
        op("act", lambda e: e.activation(out=sm["sin"][:], in_=sm["s1"][:], func=AF.Sin), reads=["s1"], writes=["sin"])
        op("act", lambda e: e.activation(out=sm["cos"][:], in_=sm["c1"][:], func=AF.Sin), reads=["c1"], writes=["cos"])
        op("dve", lambda e: e.tensor_tensor(out=sm["ar"][:], in0=sm["mag"][:], in1=sm["cos"][:], op=ALU.mult), reads=["mag", "cos"], writes=["ar"])
        op("dve", lambda e: e.tensor_tensor(out=sm["ai"][:], in0=sm["mag"][:], in1=sm["sin"][:], op=ALU.mult), reads=["mag", "sin"], writes=["ai"])
        op("dve", lambda e: V(e, sm["nai"][:], sm["ai"][:], -1.0, None, ALU.mult), reads=["ai"], writes=["nai"])
        op("dve", lambda e: V(e, c["m1"][:], sm["ar"][:, 0:1], -1.0, None, ALU.add), reads=["ar"], writes=["c_m1"])
        op("dve", lambda e, Pm=Pm: e.tensor_tensor(out=c["t1"][:], in0=Pm[:, 0:1], in1=Pm[:, 0:1], op=ALU.mult), reads=[pk], writes=["c_t1"])
        op("dve", lambda e, Pm=Pm: e.scalar_tensor_tensor(out=c["den"][:], in0=Pm[:, 1:2], scalar=Pm[:, 1:2], in1=c["t1"][:], op0=ALU.mult, op1=ALU.add),
             reads=[pk, "c_t1"], writes=["c_den"])
        op("dve", lambda e: e.reciprocal(out=c["den"][:], in_=c["den"][:]), reads=["c_den"], writes=["c_den"])
        op("dve", lambda e, Pm=Pm: e.tensor_tensor(out=c["t1"][:], in0=c["m1"][:], in1=Pm[:, 0:1], op=ALU.mult), reads=[pk, "c_m1", "c_den"], writes=["c_t1"])
        op("dve", lambda e, Pm=Pm: e.scalar_tensor_tensor(out=c["t1"][:], in0=sm["ai"][:, 0:1], scalar=Pm[:, 1:2], in1=c["t1"][:], op0=ALU.mult, op1=ALU.add),
             reads=[pk, "ai", "c_t1"], writes=["c_t1"])
        op("dve", lambda e: e.tensor_tensor(out=c["wre"][:], in0=c["t1"][:], in1=c["den"][:], op=ALU.mult), reads=["c_t1", "c_den"], writes=["c_wre"])
        op("dve", lambda e, Pm=Pm: e.tensor_tensor(out=c["t2"][:], in0=c["m1"][:], in1=Pm[:, 1:2], op=ALU.mult), reads=[pk, "c_m1"], writes=["c_t2"])
        op("dve", lambda e, Pm=Pm: e.scalar_tensor_tensor(out=c["t2"][:], in0=sm["ai"][:, 0:1], scalar=Pm[:, 0:1], in1=c["t2"][:], op0=ALU.mult, op1=ALU.subtract),
             reads=[pk, "ai", "c_t2"], writes=["c_t2"])
        op("dve", lambda e: e.tensor_tensor(out=c["wim"][:], in0=c["t2"][:], in1=c["den"][:], op=ALU.mult), reads=["c_t2", "c_den"], writes=["c_wim"])
        op("dve", lambda e: V(e, c["nwim"][:], c["wim"][:], -1.0, None, ALU.mult), reads=["c_wim"], writes=["c_nwim"])
        op("dve", lambda e, Pm=Pm: V(e, bb["bre"][:], Pm[:, 3:19], c["wre"][:, 0:1], None, ALU.mult), reads=[pk, "c_wre"], writes=["b_bre"])
        op("dve", lambda e, Pm=Pm: e.scalar_tensor_tensor(out=bb["bre"][:], in0=Pm[:, 19:35], scalar=c["nwim"][:, 0:1], in1=bb["bre"][:], op0=ALU.mult, op1=ALU.add),
             reads=[pk, "c_nwim", "b_bre"], writes=["b_bre"])
        op("dve", lambda e, Pm=Pm: V(e, bb["bim"][:], Pm[:, 19:35], c["wre"][:, 0:1], None, ALU.mult), reads=[pk, "c_wre"], writes=["b_bim"])
        op("dve", lambda e, Pm=Pm: e.scalar_tensor_tensor(out=bb["bim"][:], in0=Pm[:, 3:19], scalar=c["wim"][:, 0:1], in1=bb["bim"][:], op0=ALU.mult, op1=ALU.add),
             reads=[pk, "c_wim", "b_bim"], writes=["b_bim"])
        for n in ("bre", "bim"):
            op("pool", lambda e, n=n: e.memset(bx[n][:], 0.0), writes=["x_" + n])
            for g2 in range(2):
                op("dve", lambda e, n=n, g2=g2: e.tensor_copy(out=bx[n][g2 * 64:(g2 + 1) * 64, g2 * 16:(g2 + 1) * 16], in_=bb[n][g2 * 64:(g2 + 1) * 64, :]),
                     reads=["b_" + n, "x_" + n], writes=["x_" + n])
            op("pe", lambda e, n=n: e.transpose(out=ps_t[:], in_=bx[n][:], identity=identf[:]), reads=["x_" + n, "identf"], writes=["ps_t"])
            op("dve", lambda e, n=n: e.tensor_copy(out=lb_[n][:], in_=ps_t[:]), reads=["ps_t"], writes=["l_" + n])
        for n, c0, sgn in (("cre", 35, 1.0), ("ncim", 51, -1.0), ("ncre", 35, -1.0)):
            op("pool", lambda e, n=n: e.memset(cxs[n][:], 0.0), writes=["cx_" + n])
            for g2 in range(2):
                op("dve", lambda e, n=n, g2=g2, c0=c0, sgn=sgn, Pm=Pm: V(e, cxs[n][g2 * 64:(g2 + 1) * 64, g2 * 16:(g2 + 1) * 16],
                                                                         Pm[g2 * 64:(g2 + 1) * 64, c0:c0 + 16], sgn, None, ALU.mult),
                     reads=[pk, "cx_" + n], writes=["cx_" + n])
        op("dve", lambda e: e.tensor_scalar(out=dx[:], in0=identf[0:32, 0:32], scalar1=dsb[b][:, 0:1], scalar2=None, op0=ALU.mult),
           reads=["identf", f"dsb{b}"], writes=["dx"])
        yield
        for nm, shift, R_ in (("sinT", 0.0, sinT), ("cosT", 0.5 * PI, cosT)):
            op("dve", lambda e, R_=R_, shift=shift: V(e, R_[:], iota1[:], c["th"][:, 0:1], shift, ALU.mult, ALU.add), reads=["iota1", "c_th"], writes=[nm])
            op("dve", lambda e, R_=R_: V(e, rrT[:], R_[:], 1.0 / (2 * PI), None, ALU.mult), reads=[nm], writes=["rrT"])
            op("dve", lambda e: e.tensor_copy(out=rrI[:], in_=rrT[:]), reads=["rrT"], writes=["rrI"])
            op("dve", lambda e: e.tensor_copy(out=rrT[:], in_=rrI[:]), reads=["rrI"], writes=["rrT"])
            op("dve", lambda e, R_=R_: e.scalar_tensor_tensor(out=R_[:], in0=rrT[:], scalar=-6.28125, in1=R_[:], op0=ALU.mult, op1=ALU.add),
               reads=["rrT", nm], writes=[nm])
            op("dve", lambda e, R_=R_: e.scalar_tensor_tensor(out=R_[:], in0=rrT[:], scalar=-(2 * PI - 6.28125), in1=R_[:], op0=ALU.mult, op1=ALU.add),
               reads=["rrT", nm], writes=[nm])
            yield
            op("dve", lambda e, R_=R_: e.tensor_single_scalar(out=rrT[:], in_=R_[:], scalar=PI, op=ALU.is_gt), reads=[nm], writes=["rrT"])
            op("dve", lambda e, R_=R_: e.scalar_tensor_tensor(out=R_[:], in0=rrT[:], scalar=-2 * PI, in1=R_[:], op0=ALU.mult, op1=ALU.add),
               reads=["rrT", nm], writes=[nm])
            op("dve", lambda e, R_=R_: e.tensor_single_scalar(out=rrT[:], in_=R_[:], scalar=-PI, op=ALU.is_lt), reads=[nm], writes=["rrT"])
            op("dve", lambda e, R_=R_: e.scalar_tensor_tensor(out=R_[:], in0=rrT[:], scalar=2 * PI, in1=R_[:], op0=ALU.mult, op1=ALU.add),
               reads=["rrT", nm], writes=[nm])
            op("act", lambda e, R_=R_: e.activation(out=R_[:], in_=R_[:], func=AF.Sin), reads=[nm], writes=[nm])
            yield
        op("dve", lambda e: V(e, rT[:], iota1[:], 0.0, sm["mag"][:, 0:1], ALU.mult, ALU.add), reads=["iota1", "mag"], writes=["rT"])
        TT = lambda e, o, a, b_, o_=ALU.mult: e.tensor_tensor(out=o, in0=a, in1=b_, op=o_)
        for ch in range(NCH):
            t0 = ch * TC
            for n_i, (n, dst) in enumerate((("bre", "reA"), ("bim", "imA"))):
                pb = n_i
                op("pe", lambda e, pb=pb, n=n, b=b, t0=t0: e.matmul(ps_bu[pb][:], lhsT=lb_[n][:], rhs=ub[b][:, t0:t0 + TC], start=True, stop=True),
                   reads=["l_" + n, f"ub{b}"], writes=[f"ps_bu{pb}"])
                op("dve", lambda e, pb=pb, dst=dst: e.tensor_copy(out=st[dst][:], in_=ps_bu[pb][:]), reads=[f"ps_bu{pb}"], writes=[dst])
                if n_i == 0:
                    yield
            op("dve", lambda e: TT(e, st["reB"][:], cosT[:], st["reA"][:]), reads=["cosT", "reA"], writes=["reB"])
            op("dve", lambda e: TT(e, T1[:], sinT[:], st["imA"][:]), reads=["sinT", "imA"], writes=["T1"])
            op("dve", lambda e: TT(e, st["reB"][:], st["reB"][:], T1[:], ALU.add), reads=["reB", "T1"], writes=["reB"])
            yield
            op("dve", lambda e: TT(e, T1[:], sinT[:], st["reA"][:]), reads=["sinT", "reA"], writes=["T1"])
            op("dve", lambda e: TT(e, st["imB"][:], cosT[:], st["imA"][:]), reads=["cosT", "imA"], writes=["imB"])
            op("dve", lambda e: TT(e, st["imB"][:], st["imB"][:], T1[:], ALU.subtract), reads=["imB", "T1"], writes=["imB"])
            ini_r = c["cre"][:] if ch > 0 else 0.0
            ini_i = c["cim"][:] if ch > 0 else 0.0
            op("dve", lambda e, ini_r=ini_r: e.tensor_tensor_scan(out=zre[:], data0=rT[:], data1=st["reB"][:], initial=ini_r, op0=ALU.mult, op1=ALU.add),
               reads=["rT", "reB", "c_cre"], writes=["zre"])
            op("dve", lambda e, ini_i=ini_i: e.tensor_tensor_scan(out=zim[:], data0=rT[:], data1=st["imB"][:], initial=ini_i, op0=ALU.mult, op1=ALU.add),
               reads=["rT", "imB", "c_cim"], writes=["zim"])
            if ch + 1 < NCH:
                cl, sl = cosT[:, TC - 1:TC], sinT[:, TC - 1:TC]
                zr, zi = zre[:, TC - 1:TC], zim[:, TC - 1:TC]
                op("dve", lambda e, zi=zi, sl=sl: TT(e, c["t3"][:], zi, sl), reads=["zim", "sinT"], writes=["c_t3"])
                op("dve", lambda e, zr=zr, cl=cl: e.scalar_tensor_tensor(out=c["cre"][:], in0=zr, scalar=cl, in1=c["t3"][:], op0=ALU.mult, op1=ALU.subtract),
                   reads=["zre", "cosT", "c_t3"], writes=["c_cre"])
                op("dve", lambda e, zr=zr, sl=sl: TT(e, c["t4"][:], zr, sl), reads=["zre", "sinT"], writes=["c_t4"])
                op("dve", lambda e, zi=zi, cl=cl: e.scalar_tensor_tensor(out=c["cim"][:], in0=zi, scalar=cl, in1=c["t4"][:], op0=ALU.mult, op1=ALU.add),
                   reads=["zim", "cosT", "c_t4"], writes=["c_cim"])
            yield
            op("dve", lambda e: TT(e, st["reB"][:], cosT[:], zre[:]), reads=["cosT", "zre"], writes=["reB"])
            op("dve", lambda e: TT(e, T1[:], sinT[:], zim[:]), reads=["sinT", "zim"], writes=["T1"])
            op("dve", lambda e: TT(e, rebf[:], st["reB"][:], T1[:], ALU.subtract), reads=["reB", "T1"], writes=["rebf"])
            yield
            op("dve", lambda e: TT(e, st["imB"][:], sinT[:], zre[:]), reads=["sinT", "zre"], writes=["imB"])
            op("dve", lambda e: TT(e, T1[:], cosT[:], zim[:]), reads=["cosT", "zim"], writes=["T1"])
            op("dve", lambda e: TT(e, imbf[:], st["imB"][:], T1[:], ALU.add), reads=["imB", "T1"], writes=["imbf"])
            yield
            yi = iyc[0] % 2
            iyc[0] += 1
            for mi, (wn, rhs_, rk) in enumerate((("cre", rebf, "rebf"), ("ncim", imbf, "imbf"))):
                op("pe", lambda e, yi=yi, wn=wn, rhs_=rhs_, mi=mi: e.matmul(ps_y[yi][:], lhsT=cxs[wn][:], rhs=rhs_[:], start=(mi == 0), stop=False),
                   reads=["cx_" + wn, rk], writes=[f"ps_y{yi}"])
            op("pe", lambda e, yi=yi, b=b, t0=t0: e.matmul(ps_y[yi][:], lhsT=dx[:], rhs=ub[b][:, t0:t0 + TC], start=False, stop=True),
               reads=["dx", f"ub{b}"], writes=[f"ps_y{yi}"])
            op("dve", lambda e, yi=yi: e.tensor_copy(out=ysb[yi][:], in_=ps_y[yi][:]), reads=[f"ps_y{yi}"], writes=[f"ysb{yi}"])
            dma("sp", yT[u, :, t0:t0 + TC], ysb[yi][:], f"st_y{yi}", reads=[f"ysb{yi}"])
            yield

    def slot_gen(b):
        for u in range(b, nunits, NS):
            yield from unit_gen(u, b)

    gens = [slot_gen(b) for b in range(min(NS, nunits))]
    if defer:
        return gens
    while gens:
        for g in list(gens):
            try:
                next(g)
            except StopIteration:
                gens.remove(g)
    return p.build() if own else p.phase_end()


DFF = 2816
GC = 1.5957691216057308


def wview(w):
    return w.rearrange("(c p) n -> p c n", p=128)


def emit_ffn(p, hT, hkey, xres, NT, experts, pm, wbufs, actTs, sgt, cw=None, after_final=None):
    NTC = NT // 4
    NA = len(actTs)
    blocks = []
    for ei, (wg, wu, wd, F) in enumerate(experts):
        for fb in range(F // 256):
            blocks.append((ei, wg, wu, wd, fb))

    def down_units(k):
        ei, wg, wu, wd, fb = blocks[k]
        j = k % 2
        wdb = wbufs[j][2]
        actT = actTs[k % NA]
        ak = f"actT{k % NA}"
        units = []
        for i in range(NT):
            for dh in range(2):
                def emit(i=i, dh=dh):
                    di = 4 + (i * 2 + dh) % 2
                    pd = pm[di]
                    for fc in range(2):
                        p.op("pe", lambda e, pd=pd, fc=fc: e.matmul(pd[:], lhsT=actT[:, fc, i * 128:(i + 1) * 128], rhs=wdb[:, fc, dh * 512:(dh + 1) * 512],
                                                                   start=(fc == 0), stop=(fc == 1)),
                             reads=[f"wd{j}", f"{ak}_{fc}_{i // 4}"], writes=[f"pm{di}"])
                    xs = xres[:, i, dh * 512:(dh + 1) * 512]
                    if cw is None:
                        p.op("dve", lambda e, pd=pd, xs=xs: e.tensor_tensor(out=xs, in0=pd[:], in1=xs, op=ALU.add),
                             reads=[f"pm{di}", f"xres{i}"], writes=[f"xres{i}"])
                    else:
                        p.op("dve", lambda e, pd=pd, xs=xs: e.scalar_tensor_tensor(out=xs, in0=pd[:], scalar=cw[:, i, ei:ei + 1], in1=xs, op0=ALU.mult, op1=ALU.add),
                             reads=[f"pm{di}", f"xres{i}", f"cw{i}"], writes=[f"xres{i}"])
                units.append(emit)
        return units

    def flush_units(units, final):
        for idx, fn in enumerate(units):
            fn()
            if final and after_final is not None and idx % 2 == 1 and idx // 2 >= 1:
                after_final(idx // 2 - 1)
        if final and after_final is not None and units:
            after_final(NT - 1)

    pending = []
    for k, (ei, wg, wu, wd, fb) in enumerate(blocks):
        j = k % 2
        wgb, wub, wdb = wbufs[j]
        actT = actTs[k % NA]
        ak = f"actT{k % NA}"
        wgv, wuv = wview(wg), wview(wu)
        wdv = wd.rearrange("(c p) n -> p c n", p=128)
        p.dma("pool", wgb[:], wgv[:, :, fb * 256:(fb + 1) * 256], f"ld_wg{j}", writes=[f"wg{j}"])
        p.dma("pool", wub[:], wuv[:, :, fb * 256:(fb + 1) * 256], f"ld_wu{j}", writes=[f"wu{j}"])
        p.dma("pool", wdb[:], wdv[:, fb * 2:fb * 2 + 2, :], f"ld_wd{j}", writes=[f"wd{j}"])
        nun = 2 * NTC
        per = (len(pending) + nun - 1) // nun if pending else 0
        ui = 0
        for fc in range(2):
            for tc in range(NTC):
                gi = (fc * NTC + tc) % 2
                pg, pu = pm[gi], pm[2 + gi]
                ts_ = slice(tc * 512, (tc + 1) * 512)
                for kc in range(8):
                    p.op("pe", lambda e, pg=pg, kc=kc, fc=fc, ts_=ts_, wgb=wgb: e.matmul(pg[:], lhsT=wgb[:, kc, fc * 128:(fc + 1) * 128], rhs=hT[:, kc, ts_],
                                                                                     start=(kc == 0), stop=(kc == 7)),
                         reads=[f"wg{j}", f"{hkey}_{kc}_{tc}"], writes=[f"pm{gi}"])
                for kc in range(8):
                    p.op("pe", lambda e, pu=pu, kc=kc, fc=fc, ts_=ts_, wub=wub: e.matmul(pu[:], lhsT=wub[:, kc, fc * 128:(fc + 1) * 128], rhs=hT[:, kc, ts_],
                                                                                     start=(kc == 0), stop=(kc == 7)),
                         reads=[f"wu{j}", f"{hkey}_{kc}_{tc}"], writes=[f"pm{2 + gi}"])
                p.op("act", lambda e, pg=pg, gi=gi: e.activation(out=sgt[gi][:], in_=pg[:], func=AF.Silu), reads=[f"pm{gi}"], writes=[f"sgt{gi}"])
                p.op("dve", lambda e, pu=pu, gi=gi, fc=fc, ts_=ts_, actT=actT: e.tensor_tensor(out=actT[:, fc, ts_], in0=sgt[gi][:], in1=pu[:], op=ALU.mult),
                     reads=[f"pm{2 + gi}", f"sgt{gi}"], writes=[f"{ak}_{fc}_{tc}"])
                for fn in pending[ui * per:(ui + 1) * per]:
                    fn()
                ui += 1
        for fn in pending[ui * per:]:
            fn()
        pending = down_units(k)
        if NA == 1:
            flush_units(pending, k == len(blocks) - 1)
            pending = []
    flush_units(pending, True)


def build_phase_c(NT=TPC // 128, nsub=1, p=None):
    own = p is None
    if own:
        p = Prog()
    T = NT * 128
    NTC = NT // 4
    x = p.D("x", [nsub * T, D], F32, "ExternalInput")
    ysT = p.D("ysT", [512, nsub * T], F32, "ExternalInput")
    sbT = p.D("sbT", [512, nsub * T], BF16, "ExternalInput")
    wglu = p.D("wglu", [512, 512], F32, "ExternalInput")
    bglu = p.D("bglu", [128, 4], F32, "ExternalInput")
    wout = p.D("wout", [D, D], F32, "ExternalInput")
    g_ffn = p.D("g_ffn", [128, 8], F32, "ExternalInput")
    wg = p.D("wg", [D, DFF], F32, "ExternalInput")
    wu = p.D("wu", [D, DFF], F32, "ExternalInput")
    wd = p.D("wd", [DFF, D], F32, "ExternalInput")
    g_mix = p.D("g_mix", [128, 8], F32, "ExternalInput")
    win = p.D("win", [D, 3088], F32, "ExternalInput")
    x1 = p.D("x1", [T, D], F32, "ExternalOutput")
    featT = p.D("featT", [2048, nsub * T], BF16, "ExternalOutput")
    v1 = p.D("v1", [nsub * T, 1024], BF16, "ExternalOutput")
    flT = p.D("flT", [16, nsub * T], F32, "ExternalOutput")

    ident = ident_bf16(p)
    sc = norm_scratch(p, with_xt=False)
    xres = p.sb("xres", [128, NT, D], F32)
    bigA = p.sb("bigA", [128, 8, T], BF16)
    bigB = p.sb("bigB", [128, 8, T], BF16)
    pm = [p.ps(f"pm{i}", [128, 512], F32) for i in range(6)]
    memo = {}

    def SB(name, shape, dt):
        if name not in memo:
            memo[name] = p.sb(name, shape, dt)
        return memo[name]

    def PS(name, shape, dt=F32):
        if name not in memo:
            memo[name] = p.ps(name, shape, dt)
        return memo[name]

    gk = [0]

    def gelu_part(sub):
        T0 = sub * T
        yst = [SB(f"yst{i}", [128, 512], F32) for i in range(2)]
        gt = [SB(f"gt{i}", [128, 512], F32) for i in range(2)]
        for tc in range(NTC):
            ts_ = slice(tc * 512, (tc + 1) * 512)
            for cc in range(4):
                b = gk[0] % 2
                gk[0] += 1
                p.dma("sp", yst[b][:], ysT[cc * 128:(cc + 1) * 128, T0 + tc * 512:T0 + (tc + 1) * 512], f"ld_y{b}", writes=[f"yst{b}"])
                p.op("dve", lambda e, b=b: e.tensor_tensor(out=gt[b][:], in0=yst[b][:], in1=yst[b][:], op=ALU.mult), reads=[f"yst{b}"], writes=[f"gt{b}"])
                p.op("dve", lambda e, b=b: e.tensor_scalar(out=gt[b][:], in0=gt[b][:], scalar1=0.044715, scalar2=1.0, op0=ALU.mult, op1=ALU.add), reads=[f"gt{b}"], writes=[f"gt{b}"])
                p.op("dve", lambda e, b=b: e.tensor_tensor(out=gt[b][:], in0=gt[b][:], in1=yst[b][:], op=ALU.mult), reads=[f"gt{b}", f"yst{b}"], writes=[f"gt{b}"])
                p.op("act", lambda e, b=b: e.activation(out=gt[b][:], in_=gt[b][:], func=AF.Sigmoid, scale=GC), reads=[f"gt{b}"], writes=[f"gt{b}"])
                p.op("dve", lambda e, b=b, cc=cc, ts_=ts_: e.tensor_tensor(out=bigB[:, cc, ts_], in0=gt[b][:], in1=yst[b][:], op=ALU.mult),
                     reads=[f"gt{b}", f"yst{b}"], writes=[f"bigB_{cc}_{tc}"])
                yield

    for sub in range(nsub):
        T0 = sub * T
        gains = SB("gains", [128, 16], F32)
        bgl = SB("bgl", [128, 4], F32)
        p.dma("sp", gains[:, 0:8], g_ffn[:, :], "ld_g0", writes=["gain0"])
        p.dma("sp", gains[:, 8:16], g_mix[:, :], "ld_g1", writes=["gain1"])
        p.dma("sp", bgl[:], bglu[:, :], "ld_bg", writes=["bgl"])
        wglu_sb = SB("wglu_sb", [128, 4, 512], BF16)
        p.dma("pool", wglu_sb[:], wview(wglu), "ld_wglu", writes=["wglu"])
        wout_sb = SB("wout_sb", [128, 8, 1024], BF16)
        for h in range(2):
            p.dma("pool", wout_sb[:, :, h * 512:(h + 1) * 512], wview(wout)[:, :, h * 512:(h + 1) * 512], f"ld_wo{h}", writes=[f"wout{h}"])
        for i in range(NT):
            p.dma("sp", xres[:, i, :], x[T0 + i * 128:T0 + (i + 1) * 128, :], f"ld_x{i}", writes=[f"xres{i}"])
        for hc in range(4):
            p.dma("sp", bigA[:, 4 + hc, :], sbT[hc * 128:(hc + 1) * 128, T0:T0 + T], f"ld_sb{hc}", writes=[f"bigA_{4 + hc}_{tc}" for tc in range(NTC)])
        if sub == 0:
            for _ in gelu_part(0):
                pass
        sgt = [SB(f"sgt{i}", [128, 512], F32) for i in range(2)]
        for tc in range(NTC):
            ts_ = slice(tc * 512, (tc + 1) * 512)
            for n in range(4):
                gi = n % 2
                for cc in range(4):
                    p.op("pe", lambda e, gi=gi, cc=cc, n=n, ts_=ts_: e.matmul(pm[gi][:], lhsT=wglu_sb[:, cc, n * 128:(n + 1) * 128], rhs=bigB[:, cc, ts_],
                                                                          start=(cc == 0), stop=(cc == 3)),
                         reads=["wglu", f"bigB_{cc}_{tc}"], writes=[f"pm{gi}"])
                p.op("act", lambda e, gi=gi, n=n: e.activation(out=sgt[gi][:], in_=pm[gi][:], func=AF.Sigmoid, bias=bgl[:, n:n + 1]),
                     reads=[f"pm{gi}", "bgl"], writes=[f"sgt{gi}"])
                p.op("dve", lambda e, gi=gi, n=n, ts_=ts_: e.tensor_tensor(out=bigA[:, n, ts_], in0=bigB[:, n, ts_], in1=sgt[gi][:], op=ALU.mult),
                     reads=[f"sgt{gi}", f"bigB_{n}_{tc}"], writes=[f"bigA_{n}_{tc}"])
        for i in range(NT):
            for dh in range(2):
                di = 2 + (i * 2 + dh) % 2
                for kc in range(8):
                    p.op("pe", lambda e, di=di, kc=kc, i=i, dh=dh: e.matmul(pm[di][:], lhsT=bigA[:, kc, i * 128:(i + 1) * 128], rhs=wout_sb[:, kc, dh * 512:(dh + 1) * 512],
                                                                        start=(kc == 0), stop=(kc == 7)),
                         reads=[f"wout{dh}", f"bigA_{kc}_{i // 4}"], writes=[f"pm{di}"])
                xs = xres[:, i, dh * 512:(dh + 1) * 512]
                p.op("dve", lambda e, di=di, xs=xs: e.tensor_tensor(out=xs, in0=pm[di][:], in1=xs, op=ALU.add), reads=[f"pm{di}", f"xres{i}"], writes=[f"xres{i}"])
            if i >= 1:
                emit_norm_transpose(p, None, gains[:, 0:8], "gain0", bigB, "bigB", ident, NT, sc, xres=xres, tiles=[i - 1])
        emit_norm_transpose(p, None, gains[:, 0:8], "gain0", bigB, "bigB", ident, NT, sc, xres=xres, tiles=[NT - 1])
        wbufs = [(SB(f"wgb{j}", [128, 8, 256], BF16), SB(f"wub{j}", [128, 8, 256], BF16), SB(f"wdb{j}", [128, 2, 1024], BF16)) for j in range(2)]
        actT = SB("actT", [128, 2, T], BF16)
        emit_ffn(p, bigB, "bigB", xres, NT, [(wg, wu, wd, DFF)], pm, wbufs, [actT], sgt,
                 after_final=lambda i: emit_norm_transpose(p, None, gains[:, 8:16], "gain1", bigA, "bigA", ident, NT, sc, xres=xres, tiles=[i]))
        for i in range(NT if sub == nsub - 1 else 0):
            p.dma("sp", x1[i * 128:(i + 1) * 128, :], xres[:, i, :], f"st_x{i % 4}", reads=[f"xres{i}"])
        gelu_gen = gelu_part(sub + 1) if sub + 1 < nsub else iter(())
        winv = wview(win)
        wf = SB("wf", [128, 8, 16], BF16)
        p.dma("pool", wf[:], winv[:, :, 3072:3088], "ld_wf", writes=["wf"])
        ob = [SB(f"ob{i}", [128, 512], BF16) for i in range(4)]
        k = 0
        for blk in range(6):
            h = blk % 2
            w1 = wout_sb[:, :, h * 512:(h + 1) * 512]
            p.dma("pool", w1, winv[:, :, blk * 512:(blk + 1) * 512], f"ld_wo{h}", writes=[f"wout{h}"])
            if blk < 4:
                sc_ = 0.125 if blk < 2 else 1.0
                for n in range(4):
                    for tc in range(NTC):
                        pi_ = k % 4
                        k += 1
                        ts_ = slice(tc * 512, (tc + 1) * 512)
                        for kc in range(8):
                            p.op("pe", lambda e, pi_=pi_, kc=kc, n=n, ts_=ts_, w1=w1: e.matmul(pm[pi_][:], lhsT=w1[:, kc, n * 128:(n + 1) * 128], rhs=bigA[:, kc, ts_],
                                                                                           start=(kc == 0), stop=(kc == 7)),
                                 reads=[f"wout{h}", f"bigA_{kc}_{tc}"], writes=[f"pm{pi_}"])
                        if pi_ < 2:
                            p.op("act", lambda e, pi_=pi_, sc_=sc_: e.activation(out=ob[pi_][:], in_=pm[pi_][:], func=AF.Copy, scale=sc_), reads=[f"pm{pi_}"], writes=[f"ob{pi_}"])
                        else:
                            p.op("dve", lambda e, pi_=pi_, sc_=sc_: e.tensor_scalar(out=ob[pi_][:], in0=pm[pi_][:], scalar1=sc_, scalar2=None, op0=ALU.mult), reads=[f"pm{pi_}"], writes=[f"ob{pi_}"])
                        r0 = blk * 512 + n * 128
                        p.dma("sp", featT[r0:r0 + 128, T0 + tc * 512:T0 + (tc + 1) * 512], ob[pi_][:], f"st_ob{pi_}", reads=[f"ob{pi_}"])
                        if k % 3 == 0:
                            next(gelu_gen, None)
            else:
                for i in range(NT):
                    pi_ = k % 4
                    k += 1
                    for kc in range(8):
                        p.op("pe", lambda e, pi_=pi_, kc=kc, i=i, w1=w1: e.matmul(pm[pi_][:], lhsT=bigA[:, kc, i * 128:(i + 1) * 128], rhs=w1[:, kc, :],
                                                                              start=(kc == 0), stop=(kc == 7)),
                             reads=[f"wout{h}", f"bigA_{kc}_{i // 4}"], writes=[f"pm{pi_}"])
                    if pi_ < 2:
                        p.op("act", lambda e, pi_=pi_: e.activation(out=ob[pi_][:], in_=pm[pi_][:], func=AF.Copy), reads=[f"pm{pi_}"], writes=[f"ob{pi_}"])
                    else:
                        p.op("dve", lambda e, pi_=pi_: e.tensor_copy(out=ob[pi_][:], in_=pm[pi_][:]), reads=[f"pm{pi_}"], writes=[f"ob{pi_}"])
                    p.dma("sp", v1[T0 + i * 128:T0 + (i + 1) * 128, (blk - 4) * 512:(blk - 3) * 512], ob[pi_][:], f"st_ob{pi_}", reads=[f"ob{pi_}"])
        for _ in gelu_gen:
            pass
        for tc in range(NTC):
            ts_ = slice(tc * 512, (tc + 1) * 512)
            pi_ = 4 + tc % 2
            for kc in range(8):
                p.op("pe", lambda e, pi_=pi_, kc=kc, ts_=ts_: e.matmul(pm[pi_][0:16, :], lhsT=wf[:, kc, :], rhs=bigA[:, kc, ts_], start=(kc == 0), stop=(kc == 7)),
                     reads=["wf", f"bigA_{kc}_{tc}"], writes=[f"pm{pi_}"])
            p.op("dve", lambda e, pi_=pi_, tc=tc: e.tensor_copy(out=sgt[tc % 2][0:16, :], in_=pm[pi_][0:16, :]), reads=[f"pm{pi_}"], writes=[f"sgt{tc % 2}"])
            p.dma("sp", flT[:, T0 + tc * 512:T0 + (tc + 1) * 512], sgt[tc % 2][0:16, :], f"st_obf{tc % 2}", reads=[f"sgt{tc % 2}"])
    return p.build() if own else p.phase_end()


DFE = 3584
NEXP = 8


def build_phase_e(NT=TPC // 128, nexp=NEXP, dfe=DFE, p=None):
    own = p is None
    if own:
        p = Prog()
    T = NT * 128
    x1 = p.D("x1", [T, D], F32, "ExternalInput")
    coT = p.D("coT", [D, T], BF16, "ExternalInput")
    wout = p.D("wout", [D, D], F32, "ExternalInput")
    g_ffn = p.D("g_ffn", [128, 8], F32, "ExternalInput")
    g_row = p.D("g_row", [1, D], F32, "ExternalInput")
    wrT = p.D("wrT", [nexp, D], F32, "ExternalInput")
    mg = p.D("mg", [nexp, D, dfe], F32, "ExternalInput")
    mu = p.D("mu", [nexp, D, dfe], F32, "ExternalInput")
    md = p.D("md", [nexp, dfe, D], F32, "ExternalInput")
    gfin = p.D("gfin", [1, D], F32, "ExternalInput")
    out = p.D("out", [T, D], F32, "ExternalOutput")

    ident = ident_bf16(p)
    sc = norm_scratch(p, with_xt=False)
    xres = p.sb("xres", [128, NT, D], F32)
    bigA = p.sb("bigA", [128, 8, T], BF16)
    pm = [p.ps(f"pm{i}", [128, 512], F32) for i in range(6)]
    gains = p.sb("gains", [128, 8], F32)
    p.dma("sp", gains[:], g_ffn[:, :], "ld_g0", writes=["gain0"])
    gfb = p.sb("gfb", [128, D], F32)
    p.dma("sp", gfb[:], gfin.partition_broadcast(128), "ld_gf", writes=["gfb"])
    grb = p.sb("grb", [128, D], F32)
    p.dma("sp", grb[:], g_row.partition_broadcast(128), "ld_gr", writes=["grb"])
    wrb = p.sb("wrb", [128, nexp, D], F32)
    for e_ in range(nexp):
        p.dma("sp", wrb[:, e_, :], wrT[e_:e_ + 1, :].partition_broadcast(128), f"ld_wr{e_}", writes=[f"wrb{e_}"])
        p.op("pool", lambda e, e_=e_: e.tensor_tensor(out=wrb[:, e_, :], in0=wrb[:, e_, :], in1=grb[:], op=ALU.mult),
             reads=[f"wrb{e_}", "grb"], writes=[f"wrb{e_}"])
    wout_sb = p.sb("wout_sb", [128, 8, 1024], BF16)
    for h in range(2):
        p.dma("pool", wout_sb[:, :, h * 512:(h + 1) * 512], wview(wout)[:, :, h * 512:(h + 1) * 512], f"ld_wo{h}", writes=[f"wout{h}"])
    for i in range(NT):
        p.dma("sp", xres[:, i, :], x1[i * 128:(i + 1) * 128, :], f"ld_x{i}", writes=[f"xres{i}"])
    for kc in range(8):
        p.dma("sp", bigA[:, kc, :], coT[kc * 128:(kc + 1) * 128, :], f"ld_co{kc}", writes=[f"bigA_{kc}_{tc}" for tc in range(NT // 4)])
    for i in range(NT):
        for dh in range(2):
            di = 2 + (i * 2 + dh) % 2
            for kc in range(8):
                p.op("pe", lambda e, di=di, kc=kc, i=i, dh=dh: e.matmul(pm[di][:], lhsT=bigA[:, kc, i * 128:(i + 1) * 128], rhs=wout_sb[:, kc, dh * 512:(dh + 1) * 512],
                                                                    start=(kc == 0), stop=(kc == 7)),
                     reads=[f"wout{dh}", f"bigA_{kc}_{i // 4}"], writes=[f"pm{di}"])
            xs = xres[:, i, dh * 512:(dh + 1) * 512]
            p.op("dve", lambda e, di=di, xs=xs: e.tensor_tensor(out=xs, in0=pm[di][:], in1=xs, op=ALU.add), reads=[f"pm{di}", f"xres{i}"], writes=[f"xres{i}"])
    cw = p.sb("cw", [128, NT, nexp], F32)
    lg = p.sb("lg", [128, nexp], F32)
    mx = p.sb("mx", [128, 8], F32)
    msk = p.sb("msk", [128, nexp], F32)
    ex = p.sb("ex", [128, nexp], F32)
    nv1 = p.sb("nv1", [128, 1], F32)
    den = p.sb("den", [128, 1], F32)
    rj = p.sb("rj", [128, D], F32)
    ss, rs, junk, epsb = sc["ss"], sc["rs"], sc["junk"], sc["epsb"]
    tag = sc["tag"]
    for i in range(NT):
        xi = xres[:, i, :]
        p.op("act", lambda e, xi=xi: e.activation(out=junk[:], in_=xi, func=AF.Square, accum_out=ss[0][:]), reads=[f"xres{i}"], writes=[f"{tag}_junk", f"{tag}_ss0"])
        p.op("act", lambda e: e.activation(out=rs[0][:], in_=ss[0][:], func=AF.Sqrt, scale=1.0 / D, bias=epsb[:, 0:1]), reads=[f"{tag}_ss0", "epsb"], writes=[f"{tag}_rs0"])
        p.op("dve", lambda e: e.reciprocal(out=rs[0][:], in_=rs[0][:]), reads=[f"{tag}_rs0"], writes=[f"{tag}_rs0"])
        for e_ in range(nexp):
            p.op("dve", lambda e, xi=xi, e_=e_: e.scalar_tensor_tensor(out=rj[:], in0=xi, scalar=rs[0][:, 0:1], in1=wrb[:, e_, :], op0=ALU.mult, op1=ALU.mult,
                                                                    accum_out=lg[:, e_:e_ + 1]),
                 reads=[f"xres{i}", f"{tag}_rs0", f"wrb{e_}"], writes=["rj", "lg"])
        p.op("dve", lambda e: e.max(out=mx[:], in_=lg[:]), reads=["lg"], writes=["mx"])
        p.op("dve", lambda e: e.tensor_scalar(out=msk[:], in0=lg[:], scalar1=mx[:, 1:2], scalar2=None, op0=ALU.is_ge), reads=["lg", "mx"], writes=["msk"])
        p.op("dve", lambda e: e.tensor_scalar(out=nv1[:], in0=mx[:, 0:1], scalar1=-1.0, scalar2=None, op0=ALU.mult), reads=["mx"], writes=["nv1"])
        p.op("act", lambda e: e.activation(out=ex[:], in_=lg[:], func=AF.Exp, bias=nv1[:, 0:1]), reads=["lg", "nv1"], writes=["ex"])
        p.op("dve", lambda e: e.tensor_tensor(out=ex[:], in0=ex[:], in1=msk[:], op=ALU.mult), reads=["ex", "msk"], writes=["ex"])
        p.op("dve", lambda e: e.reduce_sum(out=den[:], in_=ex[:], axis=AX.X), reads=["ex"], writes=["den"])
        p.op("dve", lambda e: e.reciprocal(out=den[:], in_=den[:]), reads=["den"], writes=["den"])
        p.op("dve", lambda e, i=i: e.tensor_scalar(out=cw[:, i, :], in0=ex[:], scalar1=den[:, 0:1], scalar2=None, op0=ALU.mult), reads=["ex", "den"], writes=[f"cw{i}"])
    emit_norm_transpose(p, None, gains[:, 0:8], "gain0", bigA, "bigA", ident, NT, sc, xres=xres)
    wbufs = [(p.sb(f"wgb{j}", [128, 8, 256], BF16), p.sb(f"wub{j}", [128, 8, 256], BF16), p.sb(f"wdb{j}", [128, 2, 1024], BF16)) for j in range(2)]
    actTs = [p.sb(f"actT{i}", [128, 2, T], BF16) for i in range(2)]
    sgt = [p.sb(f"sgt{i}", [128, 512], BF16) for i in range(2)]
    emit_ffn(p, bigA, "bigA", xres, NT, [(mg[e_], mu[e_], md[e_], dfe) for e_ in range(nexp)], pm, wbufs, actTs, sgt, cw=cw)
    ot = [rj, grb]
    otk = ["rj", "grb"]
    for i in range(NT):
        b = i % 2
        xi = xres[:, i, :]
        p.op("act", lambda e, xi=xi, b=b: e.activation(out=junk[:], in_=xi, func=AF.Square, accum_out=ss[b][:]), reads=[f"xres{i}"], writes=[f"{tag}_junk", f"{tag}_ss{b}"])
        p.op("act", lambda e, b=b: e.activation(out=rs[b][:], in_=ss[b][:], func=AF.Sqrt, scale=1.0 / D, bias=epsb[:, 0:1]), reads=[f"{tag}_ss{b}", "epsb"], writes=[f"{tag}_rs{b}"])
        p.op("dve", lambda e, b=b: e.reciprocal(out=rs[b][:], in_=rs[b][:]), reads=[f"{tag}_rs{b}"], writes=[f"{tag}_rs{b}"])
        p.op("dve", lambda e, xi=xi, b=b: e.scalar_tensor_tensor(out=ot[b][:], in0=xi, scalar=rs[b][:, 0:1], in1=gfb[:], op0=ALU.mult, op1=ALU.mult),
             reads=[f"xres{i}", f"{tag}_rs{b}", "gfb"], writes=[otk[b]])
        p.dma("sp", out[i * 128:(i + 1) * 128, :], ot[b][:], f"st_o{b}", reads=[otk[b]])
    return p.build() if own else p.phase_end()


def build_fused():
    p = Prog()
    LL, T = L, TPC
    NSUB = LL // T
    NT = T // 128
    ext = {}

    def EI(name, shape, dt=F32):
        ext[name] = p.dram(name, list(shape), dt, "ExternalInput").ap()
        return ext[name]

    def SC(name, shape, dt):
        return p.dram("sc_" + name, list(shape), dt, "Internal").ap()

    x = EI("x", [LL, D])
    padrow = EI("padrow", [1, LL], BF16)
    g_mix0 = EI("g_mix0", [128, 8]); w_in0 = EI("w_in0", [D, 2048])
    s5prm = EI("s5prm", [16, 128, 67]); s5dd = EI("s5dd", [16, 32, 1])
    wglu = EI("wglu", [512, 512]); bglu = EI("bglu", [128, 4]); w_out0 = EI("w_out0", [D, D]); g_ffn0 = EI("g_ffn0", [128, 8])
    wg = EI("wg", [D, DFF]); wu = EI("wu", [D, DFF]); wd = EI("wd", [DFF, D])
    g_mix1 = EI("g_mix1", [128, 8]); w_in1 = EI("w_in1", [D, 3088]); negb = EI("negb", [128, 1])
    w_out1 = EI("w_out1", [D, D]); g_ffn1 = EI("g_ffn1", [128, 8]); g_row1 = EI("g_row1", [1, D]); wrT = EI("wrT", [NEXP, D])
    mg = EI("mg", [NEXP, D, DFE]); mu = EI("mu", [NEXP, D, DFE]); md = EI("md", [NEXP, DFE, D]); gfin = EI("gfin", [1, D])
    out = p.dram("out", [T, D], F32, "ExternalOutput").ap()
    featT0 = SC("featT0", [1536, LL], BF16); v0 = SC("v0", [LL, 512], BF16)
    sbT = SC("sbT", [512, LL], BF16); ysT = SC("ysT", [512, LL], F32)
    feat1T = SC("feat1T", [2048, LL], BF16); v1 = SC("v1", [LL, 1024], BF16); flT = SC("flT", [16, LL], F32)
    x1 = SC("x1", [T, D], F32); coT = SC("coT", [D, T], BF16)

    p.io = dict(x=x, gain=g_mix0, w=w_in0, featT=featT0, v=v0)
    build_phase_a(nsub=NSUB, p=p)
    p.io = dict(qT=PairView(lambda h: featT0[512 + h * 64:512 + (h + 1) * 64, :]), kT=PairView(lambda h: featT0[1024 + h * 64:1024 + (h + 1) * 64, :]),
                v=PairView(lambda h: v0[:, h * 64:(h + 1) * 64]), oT=PairView(lambda h: sbT[h * 64:(h + 1) * 64, :]))
    p.io.update(uT=PairView(lambda u: featT0[u * 32:(u + 1) * 32, :]), prm=s5prm, dd=s5dd, yT=PairView(lambda u: ysT[u * 32:(u + 1) * 32, :]))
    psx = p.ps("psx", [128, 512], F32)
    gens = build_s5(16, LL, TC=512, p=p, psx=psx, defer=True)
    build_attn("sb", 8, LL, p=p, side=gens, side_every=(8, (0, 3, 6)), n_ps_s=3)
    p.io = dict(x=x, ysT=ysT, sbT=sbT, wglu=wglu, bglu=bglu, wout=w_out0, g_ffn=g_ffn0, wg=wg, wu=wu, wd=wd, g_mix=g_mix1, win=w_in1,
                x1=x1, featT=feat1T, v1=v1, flT=flT)
    build_phase_c(NT, nsub=NSUB, p=p)
    SEG = LL * 16 // 128
    p.io = dict(qT=PairView(lambda h: feat1T[h * 64:(h + 1) * 64, :]), kT=PairView(lambda h: feat1T[1024 + h * 64:1024 + (h + 1) * 64, :]),
                v=PairView(lambda h: v1[:, h * 64:(h + 1) * 64]), oT=PairView(lambda h: coT[h * 64:(h + 1) * 64, :]),
                fl=flT.rearrange("h (s f) -> (h s) f", f=SEG), negb=negb, padrow=padrow)
    nqc = LL // 512
    build_attn("fox", 16, LL, qcs=list(range(nqc - T // 512, nqc)), p=p)
    p.io = dict(x1=x1, coT=coT, wout=w_out1, g_ffn=g_ffn1, g_row=g_row1, wrT=wrT, mg=mg, mu=mu, md=md, gfin=gfin, out=out)
    build_phase_e(NT, p=p)
    return p.build()


def fused_inputs(inp, xb_pad, padrow):
    C = np.ascontiguousarray
    return dict(x=xb_pad, padrow=padrow, g_mix0=gainT_of(inp["ev_norm_mix"][0]), w_in0=inp["ev_w_in"][0],
                s5prm=np.stack([s5_prm_of(inp, gp) for gp in range(16)]),
                s5dd=C(np.stack([inp["s5_d"][0][2 * gp:2 * gp + 2].reshape(32, 1) for gp in range(16)]).astype(np.float32)),
                wglu=inp["s5_w_glu"][0], bglu=C(inp["s5_b_glu"][0].reshape(4, 128).T), w_out0=inp["ev_w_out"][0], g_ffn0=gainT_of(inp["ev_norm_ffn"][0]),
                wg=inp["ffn_w_gate"][0], wu=inp["ffn_w_up"][0], wd=inp["ffn_w_down"][0], g_mix1=gainT_of(inp["od_norm_mix"][0]), w_in1=inp["od_w_in"][0],
                negb=C(np.repeat(-inp["fox_b_f"][0].astype(np.float32), 8).reshape(128, 1)), w_out1=inp["od_w_out"][0],
                g_ffn1=gainT_of(inp["od_norm_ffn"][0]), g_row1=C(inp["od_norm_ffn"][0].reshape(1, D)), wrT=C(inp["moe_w_router"][0].T),
                mg=inp["moe_w_gate"][0], mu=inp["moe_w_up"][0], md=inp["moe_w_down"][0], gfin=C(inp["final_norm"].reshape(1, D)))


def kernel_fused(**inp):
    inp = {k: np.asarray(v) for k, v in inp.items()}
    x = inp["x"]
    maps = []
    for c in range(NCORES):
        b, r = divmod(c, 4)
        n_real = (r + 1) * TPC
        xp = np.zeros((L, D), np.float32)
        xp[L - n_real:] = x[b, :n_real]
        pr = np.zeros((1, L), NPBF)
        pr[0, :L - n_real] = -30000.0
        maps.append(fused_inputs(inp, xp, pr))
    res = run_spmd(_prog("FUSED", build_fused), maps)
    out = np.zeros((B, L, D), np.float32)
    for c in range(NCORES):
        b, r = divmod(c, 4)
        out[b, r * TPC:(r + 1) * TPC] = res[c]["out"]
    return out


def run_spmd(nc, in_maps):
    res = run_bass_kernel_spmd(nc, in_maps, core_ids=list(range(NCORES)))
    return res.results


def gainT_of(g):
    return np.ascontiguousarray(g.reshape(8, 128).T)


def s5_prm_of(inp, gp):
    g0 = 2 * gp
    are = inp["s5_a_re"][0][g0:g0 + 2].reshape(128, 1)
    aim = inp["s5_a_im"][0][g0:g0 + 2].reshape(128, 1)
    ls = np.repeat(inp["s5_log_step"][0][g0:g0 + 2], 64).reshape(128, 1)
    bre = inp["s5_b_re"][0][g0:g0 + 2].reshape(128, 16)
    bim = inp["s5_b_im"][0][g0:g0 + 2].reshape(128, 16)
    cre = inp["s5_c_re"][0][g0:g0 + 2].transpose(0, 2, 1).reshape(128, 16)
    cim = inp["s5_c_im"][0][g0:g0 + 2].transpose(0, 2, 1).reshape(128, 16)
    return np.ascontiguousarray(np.concatenate([are, aim, ls, bre, bim, cre, cim], 1), dtype=np.float32)


_CACHE = {}


def _prog(name, fn):
    if name not in _CACHE:
        _CACHE[name] = fn()
    return _CACHE[name]


def kernel_unfused(**inp):
    inp = {k: np.asarray(v) for k, v in inp.items()}
    C = np.ascontiguousarray
    x = inp["x"].reshape(B * L, D)
    ra = run_spmd(_prog("A", build_phase_a), [{"x": C(x[c * TPC:(c + 1) * TPC]), "gain": gainT_of(inp["ev_norm_mix"][0]), "w": inp["ev_w_in"][0]}
                                              for c in range(NCORES)])
    featT = np.concatenate([r["featT"] for r in ra], axis=1)
    v0 = np.concatenate([r["v"] for r in ra], axis=0)
    maps = []
    for c in range(NCORES):
        prs = [divmod(2 * c + j, 8) for j in range(2)]
        maps.append({"qT": C(np.stack([featT[512 + h * 64:512 + (h + 1) * 64, b * L:(b + 1) * L] for b, h in prs])),
                     "kT": C(np.stack([featT[1024 + h * 64:1024 + (h + 1) * 64, b * L:(b + 1) * L] for b, h in prs])),
                     "v": C(np.stack([v0[b * L:(b + 1) * L, h * 64:(h + 1) * 64] for b, h in prs]))})
    rb1 = run_spmd(_prog("SB", lambda: build_attn("sb", 2)), maps)
    sbT = np.zeros((B, 512, L), NPBF)
    for c in range(NCORES):
        for j in range(2):
            b, h = divmod(2 * c + j, 8)
            sbT[b, h * 64:(h + 1) * 64] = rb1[c]["oT"][j]
    maps = []
    for c in range(NCORES):
        us = [divmod(4 * c + j, 16) for j in range(4)]
        maps.append({"uT": C(np.stack([featT[gp * 32:(gp + 1) * 32, b * L:(b + 1) * L] for b, gp in us])),
                     "prm": np.stack([s5_prm_of(inp, gp) for b, gp in us]),
                     "dd": C(np.stack([inp["s5_d"][0][2 * gp:2 * gp + 2].reshape(32, 1) for b, gp in us]).astype(np.float32))})
    rb2 = run_spmd(_prog("S5", lambda: build_s5(4)), maps)
    ysT = np.zeros((B, 512, L), np.float32)
    for c in range(NCORES):
        for j in range(4):
            b, gp = divmod(4 * c + j, 16)
            ysT[b, gp * 32:(gp + 1) * 32] = rb2[c]["yT"][j]
    maps = []
    for c in range(NCORES):
        b, t0 = divmod(c * TPC, L)
        maps.append(dict(x=C(x[c * TPC:(c + 1) * TPC]), ysT=C(ysT[b][:, t0:t0 + TPC]), sbT=C(sbT[b][:, t0:t0 + TPC]),
                         wglu=inp["s5_w_glu"][0], bglu=C(inp["s5_b_glu"][0].reshape(4, 128).T), wout=inp["ev_w_out"][0],
                         g_ffn=gainT_of(inp["ev_norm_ffn"][0]), wg=inp["ffn_w_gate"][0], wu=inp["ffn_w_up"][0], wd=inp["ffn_w_down"][0],
                         g_mix=gainT_of(inp["od_norm_mix"][0]), win=inp["od_w_in"][0]))
    rc = run_spmd(_prog("C", build_phase_c), maps)
    x1 = [r["x1"] for r in rc]
    feat1 = np.concatenate([r["featT"] for r in rc], axis=1)
    v1 = np.concatenate([r["v1"] for r in rc], axis=0)
    fl = np.concatenate([r["flT"] for r in rc], axis=1)
    maps = []
    for c in range(NCORES):
        prs = [divmod(4 * c + j, 16) for j in range(4)]
        maps.append({"qT": C(np.stack([feat1[h * 64:(h + 1) * 64, b * L:(b + 1) * L] for b, h in prs])),
                     "kT": C(np.stack([feat1[1024 + h * 64:1024 + (h + 1) * 64, b * L:(b + 1) * L] for b, h in prs])),
                     "v": C(np.stack([v1[b * L:(b + 1) * L, h * 64:(h + 1) * 64] for b, h in prs])),
                     "fl": C(np.stack([fl[h, b * L:(b + 1) * L] for b, h in prs]).reshape(128, -1)),
                     "negb": C(np.repeat(np.array([-inp["fox_b_f"][0][h] for b, h in prs], np.float32), 32).reshape(128, 1)),
                     "padrow": np.zeros((1, L), NPBF)})
    rd = run_spmd(_prog("FOX", lambda: build_attn("fox", 4)), maps)
    coT = np.zeros((B, 1024, L), NPBF)
    for c in range(NCORES):
        for j in range(4):
            b, h = divmod(4 * c + j, 16)
            coT[b, h * 64:(h + 1) * 64] = rd[c]["oT"][j]
    maps = []
    for c in range(NCORES):
        b, t0 = divmod(c * TPC, L)
        maps.append(dict(x1=x1[c], coT=C(coT[b][:, t0:t0 + TPC]), wout=inp["od_w_out"][0], g_ffn=gainT_of(inp["od_norm_ffn"][0]),
                         g_row=C(inp["od_norm_ffn"][0].reshape(1, D)), wrT=C(inp["moe_w_router"][0].T), mg=inp["moe_w_gate"][0],
                         mu=inp["moe_w_up"][0], md=inp["moe_w_down"][0], gfin=C(inp["final_norm"].reshape(1, D))))
    re_ = run_spmd(_prog("E", build_phase_e), maps)
    out = np.concatenate([r["out"] for r in re_], axis=0).reshape(B, L, D)
    return out.astype(np.float32)


def kernel(**inp):
    return kernel_fused(**inp)
```

```python
import numpy as np
from contextlib import ExitStack
import ml_dtypes
import concourse.bass as bass
import concourse.mybir as mybir
from concourse.bass_utils import run_bass_kernel_spmd

F32 = mybir.dt.float32
BF16 = mybir.dt.bfloat16
AF = mybir.ActivationFunctionType
ALU = mybir.AluOpType
AX = mybir.AxisListType
NPBF = ml_dtypes.bfloat16

NCORES = 8
D = 1024
B = 2
L = 8192
TPC = B * L // NCORES
EPS = 1e-6
EVAC_MODE = 'mix'
SB_SOFTPLUS = False


class PairView:
    def __init__(self, fn):
        self.fn = fn

    def __getitem__(self, key):
        return self.fn(key[0])[tuple(key[1:])]


class Prog:
    ENGS = ("pe", "act", "dve", "pool", "sp")

    def __init__(self):
        self.nc = bass.Bass("TRN2", target_bir_lowering=False)
        self.sem_es = ExitStack()
        self.es = ExitStack()
        self.ops = {e: [] for e in self.ENGS}
        self.sems = {}
        self.handles = {}
        self.val = {}
        self.nfree = {}
        self.seen = {e: {} for e in self.ENGS}
        self.st = {}
        self.phase = 0
        self.barrier = {}
        self.io = {}

    def sem(self, key, q="sp"):
        if key in self.ENGS:
            phys = key
        else:
            if key not in self.sems:
                n = self.nfree.get(q, 0)
                self.sems[key] = f"d{q}{n}"
                self.nfree[q] = n + 1
            phys = self.sems[key]
        if phys not in self.handles:
            self.handles[phys] = self.sem_es.enter_context(self.nc.semaphore("s_" + phys))
            self.val[phys] = 0
        return phys

    def sb(self, name, shape, dt):
        return self.es.enter_context(self.nc.sbuf_tensor(f"p{self.phase}_{name}", list(shape), dt))

    def ps(self, name, shape, dt=F32):
        return self.es.enter_context(self.nc.psum_tensor(f"p{self.phase}_{name}", list(shape), dt))

    def dram(self, name, shape, dt, kind):
        return self.nc.dram_tensor(name, list(shape), dt, kind=kind)

    def D(self, name, shape, dt, kind):
        if name in self.io:
            return self.io[name]
        return self.dram(name, shape, dt, kind).ap()

    def _deps(self, eng, reads, writes):
        need = {}

        def add(sv):
            if sv is None:
                return
            k, v = sv
            if need.get(k, 0) < v:
                need[k] = v

        for r in reads:
            s = self.st.get(r)
            if s:
                add(s[0])
        for w in writes:
            s = self.st.get(w)
            if s:
                add(s[0])
                for k, v in s[1].items():
                    add((k, v))
        waits = []
        for k, v in need.items():
            if eng == "pe" and k == "pe":
                continue
            if self.seen[eng].get(k, 0) >= v:
                continue
            self.seen[eng][k] = v
            waits.append((k, v))
        return waits

    def _commit(self, semkey, val, reads, writes):
        for r in reads:
            s = self.st.setdefault(r, [None, {}])
            s[1][semkey] = val
        for w in writes:
            self.st[w] = [(semkey, val), {}]

    LIMIT = None
    NOPS = 0

    def op(self, eng, fn, reads=(), writes=()):
        Prog.NOPS += 1
        if Prog.LIMIT is not None and Prog.NOPS > Prog.LIMIT:
            return
        waits = self._deps(eng, reads, writes)
        self.sem(eng)
        self.val[eng] += 1
        v = self.val[eng]
        self._commit(eng, v, reads, writes)
        self.ops[eng].append((waits, fn, eng, 1))

    def dma(self, q, out, in_, semkey, reads=(), writes=(), **kw):
        Prog.NOPS += 1
        if Prog.LIMIT is not None and Prog.NOPS > Prog.LIMIT:
            return
        waits = self._deps(q, reads, writes)
        sk = self.sem(semkey, q)
        self.val[sk] += 16
        v = self.val[sk]
        self._commit(sk, v, reads, writes)
        self.ops[q].append((waits, lambda e: e.dma_start(out=out, in_=in_, **kw), sk, 16))

    def phase_end(self, final=False):
        nc = self.nc
        barrier = [(k, v) for k, v in self.barrier.items() if v > 0]
        fin = [(k, v) for k, v in self.val.items() if v > 0] if final else []

        def run(name):
            def f(e):
                for k, v in barrier:
                    e.wait_ge(self.handles[k], v)
                for waits, fn, sk, inc in self.ops[name]:
                    for k, v in waits:
                        e.wait_ge(self.handles[k], v)
                    fn(e).then_inc(self.handles[sk], inc)
                if name == "sp":
                    for k, v in fin:
                        e.wait_ge(self.handles[k], v)
            return f

        with nc.Block() as block:
            block.tensor(run("pe"))
            block.scalar(run("act"))
            block.vector(run("dve"))
            block.gpsimd(run("pool"))
            block.sync(run("sp"))
        self.es.close()
        self.es = ExitStack()
        self.ops = {e: [] for e in self.ENGS}
        self.barrier = dict(self.val)
        self.seen = {e: dict(self.val) for e in self.ENGS}
        self.st = {}
        self.sems = {}
        self.nfree = {}
        self.phase += 1

    def build(self):
        self.phase_end(final=True)
        self.sem_es.close()
        return self.nc


def ident_bf16(p, name="ident"):
    idf = p.sb(name + "_f", [128, 128], F32)
    idb = p.sb(name, [128, 128], BF16)
    p.op("pool", lambda e: e.memset(idf[:], 1.0), writes=[name + "_f"])
    p.op("pool", lambda e: e.affine_select(out=idf[:], in_=idf[:], pattern=[[1, 128]],
                                           compare_op=ALU.is_equal, fill=0.0, base=0,
                                           channel_multiplier=-1),
         reads=[name + "_f"], writes=[name + "_f"])
    p.op("dve", lambda e: e.tensor_copy(out=idb[:], in_=idf[:]), reads=[name + "_f"], writes=[name])
    return idb


def norm_scratch(p, tag="N", with_xt=True):
    sc = {}
    sc["xt"] = [p.sb(f"{tag}_xt{i}", [128, D], F32) for i in range(2)] if with_xt else None
    sc["junk"] = p.sb(f"{tag}_junk", [128, D], F32)
    sc["hn"] = [p.sb(f"{tag}_hn{i}", [128, D], BF16) for i in range(2)]
    sc["ss"] = [p.sb(f"{tag}_ss{i}", [128, 1], F32) for i in range(2)]
    sc["rs"] = [p.sb(f"{tag}_rs{i}", [128, 1], F32) for i in range(2)]
    sc["pst"] = [p.ps(f"{tag}_pst{i}", [128, D], BF16) for i in range(2)]
    sc["epsb"] = p.sb(f"{tag}_epsb", [128, 1], F32)
    sc["tag"] = tag
    p.op("pool", lambda e: e.memset(sc["epsb"][:], EPS), writes=["epsb"])
    return sc


def emit_norm_transpose(p, x_dram, gainT, gkey, hT, hkey, ident, ntiles, sc, xres=None, tiles=None):
    tag = sc["tag"]
    xt, junk, hn, ss, rs, pst, epsb = sc["xt"], sc["junk"], sc["hn"], sc["ss"], sc["rs"], sc["pst"], sc["epsb"]
    for i in (range(ntiles) if tiles is None else tiles):
        b = i % 2
        if xres is None:
            p.dma("sp", xt[b][:], x_dram[i * 128:(i + 1) * 128, :], f"{tag}_ld{b}", writes=[f"{tag}_xt{b}"])
            srcap = xt[b][:]
            rk = f"{tag}_xt{b}"
        else:
            srcap = xres[:, i, :]
            rk = f"xres{i}"
        p.op("act", lambda e, srcap=srcap, b=b: e.activation(out=junk[:], in_=srcap, func=AF.Square, accum_out=ss[b][:]),
             reads=[rk], writes=[f"{tag}_junk", f"{tag}_ss{b}"])
        p.op("act", lambda e, b=b: e.activation(out=rs[b][:], in_=ss[b][:], func=AF.Sqrt, scale=1.0 / D, bias=epsb[:, 0:1]),
             reads=[f"{tag}_ss{b}", "epsb"], writes=[f"{tag}_rs{b}"])
        p.op("dve", lambda e, b=b: e.reciprocal(out=rs[b][:], in_=rs[b][:]), reads=[f"{tag}_rs{b}"], writes=[f"{tag}_rs{b}"])
        p.op("dve", lambda e, srcap=srcap, b=b: e.tensor_scalar(out=hn[b][:], in0=srcap, scalar1=rs[b][:, 0:1], scalar2=None, op0=ALU.mult),
             reads=[rk, f"{tag}_rs{b}"], writes=[f"{tag}_hn{b}"])
        for c in range(8):
            p.op("pe", lambda e, b=b, c=c: e.transpose(out=pst[b][:, c * 128:(c + 1) * 128], in_=hn[b][:, c * 128:(c + 1) * 128], identity=ident[:]),
                 reads=[f"{tag}_hn{b}", "ident"], writes=[f"{tag}_pst{b}"])
        for c in range(8):
            eng = "act" if b == 0 else "dve"
            if eng == "act":
                fn = lambda e, b=b, c=c, i=i: e.activation(out=hT[:, c, i * 128:(i + 1) * 128], in_=pst[b][:, c * 128:(c + 1) * 128],
                                                           func=AF.Copy, scale=gainT[:, c:c + 1])
            else:
                fn = lambda e, b=b, c=c, i=i: e.tensor_scalar(out=hT[:, c, i * 128:(i + 1) * 128], in0=pst[b][:, c * 128:(c + 1) * 128],
                                                              scalar1=gainT[:, c:c + 1], scalar2=None, op0=ALU.mult)
            p.op(eng, fn, reads=[f"{tag}_pst{b}", gkey], writes=[f"{hkey}_{c}_{i // 4}"])


def build_phase_a(nsub=1, p=None):
    own = p is None
    if own:
        p = Prog()
    nc = p.nc
    x = p.D("x", [nsub * TPC, D], F32, "ExternalInput")
    gain = p.D("gain", [128, 8], F32, "ExternalInput")
    w = p.D("w", [D, 2048], F32, "ExternalInput")
    featT = p.D("featT", [1536, nsub * TPC], BF16, "ExternalOutput")
    vout = p.D("v", [nsub * TPC, 512], BF16, "ExternalOutput")
    NT = TPC // 128
    ident = ident_bf16(p)
    gainT = p.sb("gainT", [128, 8], F32)
    p.dma("sp", gainT[:], gain[:, :], "ld_gain", writes=["A_gain"])
    sc = norm_scratch(p)
    wsb = p.sb("wsb", [128, 8, 2048], BF16)
    wv = w.rearrange("(c p) n -> p c n", p=128)
    for c in range(8):
        p.dma("pool", wsb[:, c, :], wv[:, c, :], f"ld_w{c}", writes=[f"w{c}"])
    hTs = [p.sb(f"hT{j}", [128, 8, TPC], BF16) for j in range(2 if nsub > 1 else 1)]
    ob = [p.sb(f"ob{i}", [128, 512], BF16) for i in range(3)]
    pm = [p.ps(f"pm{i}", [128, 512], F32) for i in range(4)]
    k = 0

    def norm_tiles(sub, i0, i1):
        j = sub % len(hTs)
        emit_norm_transpose(p, x[sub * TPC:(sub + 1) * TPC, :], gainT, "A_gain", hTs[j], f"A_hT{j}", ident, NT, sc, tiles=range(i0, i1))

    norm_tiles(0, 0, NT)
    for sub in range(nsub):
        hT = hTs[sub % len(hTs)]
        hk = f"A_hT{sub % len(hTs)}"
        nxt = 0
        for n in range(12):
            if sub + 1 < nsub and n > 0:
                cnt = 2 if n <= 5 else 1
                norm_tiles(sub + 1, nxt, min(NT, nxt + cnt))
                nxt = min(NT, nxt + cnt)
            for t in range(TPC // 512):
                pb = k % 4
                sbi = k % 3
                for c in range(8):
                    p.op("pe", lambda e, pb=pb, c=c, n=n, t=t, hT=hT: e.matmul(pm[pb][:], lhsT=wsb[:, c, n * 128:(n + 1) * 128],
                                                                        rhs=hT[:, c, t * 512:(t + 1) * 512],
                                                                        start=(c == 0), stop=(c == 7)),
                         reads=[f"w{c}", f"{hk}_{c}_{t}"], writes=[f"pm{pb}"])
                qs_ = 0.125 if 4 <= n < 8 else 1.0
                if k % 2 == 0:
                    p.op("act", lambda e, pb=pb, sbi=sbi, qs_=qs_: e.activation(out=ob[sbi][:], in_=pm[pb][:], func=AF.Copy, scale=qs_),
                         reads=[f"pm{pb}"], writes=[f"ob{sbi}"])
                else:
                    p.op("dve", lambda e, pb=pb, sbi=sbi, qs_=qs_: e.tensor_scalar(out=ob[sbi][:], in0=pm[pb][:], scalar1=qs_, scalar2=None, op0=ALU.mult),
                         reads=[f"pm{pb}"], writes=[f"ob{sbi}"])
                p.dma("sp", featT[n * 128:(n + 1) * 128, sub * TPC + t * 512:sub * TPC + (t + 1) * 512], ob[sbi][:], f"st_ob{sbi}", reads=[f"ob{sbi}"])
                k += 1
        if sub + 1 < nsub and nxt < NT:
            norm_tiles(sub + 1, nxt, NT)
        for i in range(NT):
            pb = k % 4
            sbi = k % 3
            for c in range(8):
                p.op("pe", lambda e, pb=pb, c=c, i=i, hT=hT: e.matmul(pm[pb][:], lhsT=hT[:, c, i * 128:(i + 1) * 128],
                                                               rhs=wsb[:, c, 1536:2048], start=(c == 0), stop=(c == 7)),
                     reads=[f"w{c}", f"{hk}_{c}_{i // 4}"], writes=[f"pm{pb}"])
            if k % 2 == 0:
                p.op("act", lambda e, pb=pb, sbi=sbi: e.activation(out=ob[sbi][:], in_=pm[pb][:], func=AF.Copy),
                     reads=[f"pm{pb}"], writes=[f"ob{sbi}"])
            else:
                p.op("dve", lambda e, pb=pb, sbi=sbi: e.tensor_copy(out=ob[sbi][:], in_=pm[pb][:]),
                     reads=[f"pm{pb}"], writes=[f"ob{sbi}"])
            p.dma("sp", vout[sub * TPC + i * 128:sub * TPC + (i + 1) * 128, :], ob[sbi][:], f"st_ob{sbi}", reads=[f"ob{sbi}"])
            k += 1
    return p.build() if own else p.phase_end()


def build_attn(mode, npairs, LL=L, qcs=None, p=None, side=None, side_every=4, n_ps_s=3):
    own = p is None
    if own:
        p = Prog()
    fox = mode == "fox"
    NKB = LL // 128
    NQC = LL // 512
    KA = 69 if fox else 64
    QCS = list(qcs) if qcs is not None else list(range(LL // 512))
    VW = 65 if fox else 64
    qT = p.D("qT", [npairs, 64, LL], BF16, "ExternalInput")
    kT = p.D("kT", [npairs, 64, LL], BF16, "ExternalInput")
    v = p.D("v", [npairs, LL, 64], BF16, "ExternalInput")
    oT = p.D("oT", [npairs, 64, len(QCS) * 512], BF16, "ExternalOutput")
    qa = [p.sb(f"qa{i}", [KA, LL], BF16) for i in range(2)]
    ka = [p.sb(f"ka{i}", [KA, LL], BF16) for i in range(2)]
    va = [p.sb(f"va{i}", [128, NKB, VW], BF16) for i in range(2)]
    NPT = 4
    pt = [p.sb(f"pt{i}", [128, 512], BF16) for i in range(NPT)]
    ptd = [p.sb(f"ptd{j}", [128, 512], BF16) for j in range(4)]
    for j in range(4):
        p.op("pool", lambda e, j=j: e.memset(ptd[j][:], 0.0), writes=[f"ptd{j}"])
    if not fox:
        n_ps_s = 5
    ps_s = [p.ps(f"ps_s{i}", [128, 512], F32) for i in range(n_ps_s)]
    ps_o = [p.ps(f"ps_o{i}", [128, 512], F32) for i in range(2)]
    osb = p.sb("osb", [VW, 512], F32)
    ob = [p.sb(f"ob{i}", [64, 512], BF16) for i in range(2)]
    if fox:
        ps_b = p.ps("ps_b", [128, 512], F32)
        rinv = p.sb("rinv", [VW, 512], F32)
        onesf = p.sb("onesf", [VW, 64], F32)
        p.op("pool", lambda e: e.memset(onesf[:], 1.0), writes=["onesf"])
        SEG = LL * npairs // 128
        SPP = 128 // npairs
        fl_d = p.D("fl", [128, SEG], F32, "ExternalInput")
        padrow = p.D("padrow", [1, LL], BF16, "ExternalInput")
        nb_d = p.D("negb", [128, 1], F32, "ExternalInput")
        fl = p.sb("fl_sb", [128, SEG], F32)
        cn = p.sb("cn", [128, SEG], F32)
        nb = p.sb("nb", [128, 1], F32)
        tot = p.sb("tot", [128, 1], F32)
        off = p.sb("off", [128, 1], F32)
        tri = p.sb("tri", [128, 128], F32)
        hi = p.sb("hi", [128, SEG], BF16)
        nhi = p.sb("nhi", [128, SEG], BF16)
        mid = p.sb("mid", [128, SEG], BF16)
        lo = p.sb("lo", [128, SEG], BF16)
        ps_c = p.ps("ps_c", [128, 1], F32)
        p.dma("sp", fl[:], fl_d[:, :], "ld_fl", writes=["fl"])
        p.dma("sp", nb[:], nb_d[:, :], "ld_nb", writes=["nb"])
        p.op("pool", lambda e: e.memset(tri[:], 1.0), writes=["tri"])
        p.op("pool", lambda e: e.affine_select(out=tri[:], in_=tri[:], pattern=[[1, 128]], compare_op=ALU.is_gt,
                                               fill=0.0, base=0, channel_multiplier=-1), reads=["tri"], writes=["tri"])
        for a in range(1, npairs):
            p.op("pool", lambda e, a=a: e.affine_select(out=tri[:, a * SPP:(a + 1) * SPP], in_=tri[:, a * SPP:(a + 1) * SPP],
                                                        pattern=[[0, SPP]], compare_op=ALU.is_ge, fill=0.0,
                                                        base=-a * SPP, channel_multiplier=1), reads=["tri"], writes=["tri"])
        p.op("act", lambda e: e.activation(out=fl[:], in_=fl[:], func=AF.Exp, scale=-1.0, bias=nb[:, 0:1]),
             reads=["fl", "nb"], writes=["fl"])
        p.op("act", lambda e: e.activation(out=fl[:], in_=fl[:], func=AF.Ln, bias=onesf[:, 0:1] if False else 1.0),
             reads=["fl"], writes=["fl"])
        p.op("dve", lambda e: e.tensor_scalar(out=fl[:], in0=fl[:], scalar1=0.5, scalar2=None, op0=ALU.mult),
             reads=["fl"], writes=["fl"])
        p.op("dve", lambda e: e.tensor_tensor_scan(out=cn[:], data0=fl[:], data1=fl[:], initial=0.0,
                                                   op0=ALU.add, op1=ALU.add), reads=["fl"], writes=["cn"])
        p.op("dve", lambda e: e.tensor_copy(out=tot[:], in_=cn[:, SEG - 1:SEG]), reads=["cn"], writes=["tot"])
        p.op("pe", lambda e: e.matmul(ps_c[:], lhsT=tri[:], rhs=tot[:], start=True, stop=True),
             reads=["tri", "tot"], writes=["ps_c"])
        p.op("dve", lambda e: e.tensor_copy(out=off[:], in_=ps_c[:]), reads=["ps_c"], writes=["off"])
        p.op("dve", lambda e: e.tensor_scalar(out=cn[:], in0=cn[:], scalar1=off[:, 0:1], scalar2=None, op0=ALU.add),
             reads=["cn", "off"], writes=["cn"])
        p.op("dve", lambda e: e.tensor_copy(out=hi[:], in_=cn[:]), reads=["cn"], writes=["hi"])
        p.op("dve", lambda e: e.tensor_scalar(out=nhi[:], in0=hi[:], scalar1=-1.0, scalar2=None, op0=ALU.mult),
             reads=["hi"], writes=["nhi"])
        p.op("dve", lambda e: e.tensor_tensor(out=cn[:], in0=cn[:], in1=hi[:], op=ALU.subtract), reads=["cn", "hi"], writes=["cn"])
        p.op("dve", lambda e: e.tensor_copy(out=mid[:], in_=cn[:]), reads=["cn"], writes=["mid"])
        p.op("dve", lambda e: e.tensor_tensor(out=cn[:], in0=cn[:], in1=mid[:], op=ALU.subtract), reads=["cn", "mid"], writes=["cn"])
        p.op("dve", lambda e: e.tensor_copy(out=lo[:], in_=cn[:]), reads=["cn"], writes=["lo"])
        cx = p.dram("cx", [4, 128 * SEG], BF16, "Internal").ap()
        cxf = cx
        for r, src, nm in ((0, nhi, "nhi"), (1, hi, "hi"), (2, mid, "mid"), (3, lo, "lo")):
            p.dma("sp", cx[r, :].rearrange("(q f) -> q f", f=SEG), src[:], f"st_cx{r}", reads=[nm], writes=[f"cx{r}"])
    else:
        ntri_f = p.sb("ntri_f", [128, 128], F32)
        ntri = p.sb("ntri", [128, 128], BF16)
        nones = p.sb("nones", [128, 128], BF16)
        p.op("pool", lambda e: e.memset(ntri_f[:], -1.0), writes=["ntri_f"])
        p.op("pool", lambda e: e.affine_select(out=ntri_f[:], in_=ntri_f[:], pattern=[[-1, 128]], compare_op=ALU.is_ge,
                                               fill=0.0, base=0, channel_multiplier=1), reads=["ntri_f"], writes=["ntri_f"])
        p.op("dve", lambda e: e.tensor_copy(out=ntri[:], in_=ntri_f[:]), reads=["ntri_f"], writes=["ntri"])
        p.op("pool", lambda e: e.memset(nones[:], -1.0), writes=["nones"])
        LSr = [p.sb(f"LS{i}", [128, 512], BF16) for i in range(2)]
        lb = [p.sb(f"lb{i}", [128, 512], BF16) for i in range(4)]
        et = [p.sb(f"et{i}", [128, 512], F32) for i in range(2)]

    def load_pair(pi):
        if pi >= npairs:
            return
        b = pi % 2
        p.dma("sp", qa[b][0:64, :], qT[pi, :, :], f"ld_q{b}", writes=[f"qa{b}"])
        p.dma("sp", ka[b][0:64, :], kT[pi, :, :], f"ld_k{b}", writes=[f"ka{b}"])
        p.dma("sp", va[b][:, :, 0:64], v[pi, :, :].rearrange("(kb p) d -> p kb d", p=128), f"ld_v{b}", writes=[f"va{b}"])
        if fox:
            p.op("pool", lambda e, b=b: e.memset(va[b][:, :, 64:65], 1.0), reads=[], writes=[f"va1_{b}"])
            p.op("pool", lambda e, b=b: e.memset(qa[b][64:69, :], 1.0), writes=[f"qax{b}"])
            p.op("pool", lambda e, b=b: e.memset(ka[b][64:69, :], 1.0), writes=[f"kax{b}"])
            p.dma("sp", qa[b][64:65, :], cxf[0:1, pi * LL:(pi + 1) * LL], f"ld_qx{b}", reads=["cx0"], writes=[f"qax{b}"])
            for r in (65, 66, 67):
                p.dma("sp", ka[b][r:r + 1, :], cxf[r - 64:r - 63, pi * LL:(pi + 1) * LL], f"ld_kx{b}_{r}", reads=[f"cx{r - 64}"], writes=[f"kax{b}"])
            p.dma("sp", ka[b][68:69, :], padrow[0:1, :], f"ld_kx{b}_68", writes=[f"kax{b}"])

    tiles = []
    for pi in range(npairs):
        for qi_, qc in enumerate(QCS):
            nkb = 4 * qc + 4
            order = list(range(nkb)) if fox else list(range(nkb - 1, -1, -1))
            for n_i, kb in enumerate(order):
                j = kb - 4 * qc
                diag = j >= 0
                c0 = 128 * j if diag else 0
                tiles.append(dict(pi=pi, b=pi % 2, qc=qc, kb=kb, n_i=n_i, nkb=nkb, j=j, diag=diag, c0=c0, gid=pi * len(QCS) + qi_, qi=qi_,
                                  last_of_pair=(qi_ == len(QCS) - 1 and n_i == nkb - 1)))
    NTILES = len(tiles)
    ring = {"pt": 0, "lb": 0}
    deferred = {}

    def keys_of(t):
        b = t["b"]
        qk = [f"qa{b}"] + ([f"qax{b}"] if fox else [])
        kk = [f"ka{b}"] + ([f"kax{b}"] if fox else [])
        vk = [f"va{b}"] + ([f"va1_{b}"] if fox else [])
        return qk, kk, vk

    def slices_of(t):
        cs = slice(t["c0"], 512)
        qs = slice(t["qc"] * 512 + t["c0"], (t["qc"] + 1) * 512)
        ks = slice(t["kb"] * 128, (t["kb"] + 1) * 128)
        return cs, qs, ks

    def alloc_P(t):
        if t["diag"]:
            t["P"], t["pk"] = ptd[t["j"]], f"ptd{t['j']}"
        else:
            t["P"], t["pk"] = pt[ring["pt"] % NPT], f"pt{ring['pt'] % NPT}"
            ring["pt"] += 1

    def mask_P(t):
        if t["diag"]:
            P, pk, c0 = t["P"], t["pk"], t["c0"]
            p.op("pool", lambda e, P=P, c0=c0: e.affine_select(out=P[:, c0:c0 + 128], in_=P[:, c0:c0 + 128], pattern=[[1, 128]],
                                                              compare_op=(ALU.is_ge if fox else ALU.is_gt), fill=0.0, base=0, channel_multiplier=-1),
                 reads=[pk], writes=[pk])

    def stage_A(i, t):
        b = t["b"]
        qk, kk, vk = keys_of(t)
        cs, qs, ks = slices_of(t)
        si = i % n_ps_s
        pss = ps_s[si]
        t["pss"], t["psk"] = pss, f"ps_s{si}"
        p.op("pe", lambda e, pss=pss, cs=cs, ks=ks, qs=qs, b=b: e.matmul(pss[:, cs], lhsT=ka[b][:, ks], rhs=qa[b][:, qs], start=True, stop=True),
             reads=qk + kk, writes=[f"ps_s{si}"])
        if fox:
            alloc_P(t)
            P, pk = t["P"], t["pk"]
            p.op("act", lambda e, P=P, pss=pss, cs=cs: e.activation(out=P[:, cs], in_=pss[:, cs], func=AF.Exp), reads=[f"ps_s{si}"], writes=[pk])
            mask_P(t)
        else:
            li = ring["lb"] % 4
            ring["lb"] += 1
            ei = i % 2
            L_, lk = lb[li], f"lb{li}"
            E_, ek = et[ei], f"et{ei}"
            t["L"], t["lk"] = L_, lk
            if SB_SOFTPLUS:
                p.op("act", lambda e, L_=L_, pss=pss, cs=cs: e.activation(out=L_[:, cs], in_=pss[:, cs], func=AF.Softplus), reads=[f"ps_s{si}"], writes=[lk])
            else:
                p.op("act", lambda e, E_=E_, pss=pss, cs=cs: e.activation(out=E_[:, cs], in_=pss[:, cs], func=AF.Exp), reads=[f"ps_s{si}"], writes=[ek])
                p.op("act", lambda e, E_=E_, L_=L_, cs=cs: e.activation(out=L_[:, cs], in_=E_[:, cs], func=AF.Ln, bias=1.0), reads=[ek], writes=[lk])
            if t["diag"]:
                c0 = t["c0"]
                p.op("pool", lambda e, L_=L_, c0=c0: e.affine_select(out=L_[:, c0:c0 + 128], in_=L_[:, c0:c0 + 128], pattern=[[1, 128]],
                                                                    compare_op=ALU.is_gt, fill=0.0, base=0, channel_multiplier=-1), reads=[lk], writes=[lk])

    def stage_B_sb(i, t):
        cs, qs, ks = slices_of(t)
        L_, lk = t["L"], t["lk"]
        pa, pak = t["pss"], t["psk"]
        alloc_P(t)
        P, pk = t["P"], t["pk"]
        first = t["n_i"] == 0
        p.op("pe", lambda e, pa=pa, cs=cs, L_=L_, first=first: e.matmul(pa[:, cs], lhsT=ntri[:], rhs=L_[:, cs], start=False, stop=first, skip_group_check=True), reads=["ntri", lk], writes=[pak])
        li_ = t["n_i"] % 2
        LS, LSk = LSr[li_], f"LS{li_}"
        LSn, LSnk = LSr[1 - li_], f"LS{1 - li_}"
        if not first:
            p.op("pe", lambda e, pa=pa, cs=cs, LS=LS: e.matmul(pa[:, cs], lhsT=nones[:], rhs=LS[:, cs], start=False, stop=True, skip_group_check=True),
                 reads=["nones", LSk], writes=[pak])
        p.op("act", lambda e, P=P, pa=pa, cs=cs: e.activation(out=P[:, cs], in_=pa[:, cs], func=AF.Exp), reads=[pak], writes=[pk])
        mask_P(t)
        if t["n_i"] + 1 < t["nkb"]:
            c0 = t["c0"]
            if c0 > 0:
                p.op("pool", lambda e, LSn=LSn, c0=c0: e.memset(LSn[:, 0:c0], 0.0), writes=[LSnk])
            if first:
                p.op("pool", lambda e, LSn=LSn, L_=L_, cs=cs: e.tensor_copy(out=LSn[:, cs], in_=L_[:, cs]), reads=[lk, LSnk], writes=[LSnk])
            else:
                p.op("pool", lambda e, LSn=LSn, LS=LS, L_=L_, cs=cs: e.tensor_tensor(out=LSn[:, cs], in0=LS[:, cs], in1=L_[:, cs], op=ALU.add),
                     reads=[lk, LSk, LSnk], writes=[LSnk])

    def finalize_1(t):
        po, pok = ps_o[t["gid"] % 2], f"ps_o{t['gid'] % 2}"
        obi = t["gid"] % 2
        if fox:
            p.op("act", lambda e, po=po: e.activation(out=osb[:], in_=po[0:VW, :], func=AF.Copy), reads=[pok], writes=["osb"])
            p.op("dve", lambda e: e.reciprocal(out=rinv[64:65, :], in_=osb[64:65, :]), reads=["osb"], writes=["rinv"])
        else:
            p.op("act", lambda e, po=po, obi=obi: e.activation(out=ob[obi][:], in_=po[0:64, :], func=AF.Copy), reads=[pok], writes=[f"ob{obi}"])
            p.dma("sp", oT[t["pi"], :, t["qi"] * 512:(t["qi"] + 1) * 512], ob[obi][:], f"st_o{obi}", reads=[f"ob{obi}"])

    def finalize_2(t):
        obi = t["gid"] % 2
        p.op("pe", lambda e: e.matmul(ps_b[0:64, :], lhsT=onesf[64:65, :], rhs=rinv[64:65, :], start=True, stop=True), reads=["onesf", "rinv"], writes=["ps_b"])
        p.op("dve", lambda e, obi=obi: e.tensor_tensor(out=ob[obi][:], in0=osb[0:64, :], in1=ps_b[0:64, :], op=ALU.mult), reads=["osb", "ps_b"], writes=[f"ob{obi}"])
        p.dma("sp", oT[t["pi"], :, t["qi"] * 512:(t["qi"] + 1) * 512], ob[obi][:], f"st_o{obi}", reads=[f"ob{obi}"])

    def stage_PV(i, t):
        b = t["b"]
        qk, kk, vk = keys_of(t)
        po, pok = ps_o[t["gid"] % 2], f"ps_o{t['gid'] % 2}"
        P, pk, kb, n_i, nkb = t["P"], t["pk"], t["kb"], t["n_i"], t["nkb"]
        p.op("pe", lambda e, po=po, P=P, kb=kb, b=b, n_i=n_i, nkb=nkb: e.matmul(po[0:VW, :], lhsT=va[b][:, kb, :], rhs=P[:, :], start=(n_i == 0), stop=(n_i == nkb - 1)),
             reads=vk + [pk], writes=[pok])
        if n_i == nkb - 1:
            finalize_1(t)
            if fox:
                deferred.setdefault(i + 2, []).append(lambda t=t: finalize_2(t))
        if t["last_of_pair"]:
            load_pair(t["pi"] + 2)

    SK1 = 2
    SK2 = 2 if fox else 4
    rr = [0]
    load_pair(0)
    load_pair(1)
    for i in range(NTILES + SK2 + 3):
        if i < NTILES:
            stage_A(i, tiles[i])
        if not fox and 0 <= i - SK1 < NTILES:
            stage_B_sb(i, tiles[i - SK1])
        if 0 <= i - SK2 < NTILES:
            stage_PV(i, tiles[i - SK2])
        for fn in deferred.pop(i, []):
            fn()
        if side and (i % side_every == 0 if isinstance(side_every, int) else (i % side_every[0]) in side_every[1]):
            g = side[rr[0] % len(side)]
            rr[0] += 1
            try:
                next(g)
            except StopIteration:
                side.remove(g)
    assert not deferred
    while side:
        for g in list(side):
            try:
                next(g)
            except StopIteration:
                side.remove(g)
    return p.build() if own else p.phase_end()


PI = float(np.pi)


def ident_f32(p, name="identf"):
    idf = p.sb(name, [128, 128], F32)
    p.op("pool", lambda e: e.memset(idf[:], 1.0), writes=[name])
    p.op("pool", lambda e: e.affine_select(out=idf[:], in_=idf[:], pattern=[[1, 128]], compare_op=ALU.is_equal,
                                           fill=0.0, base=0, channel_multiplier=-1), reads=[name], writes=[name])
    return idf


def build_s5(nunits, LL=L, TC=2048, p=None, psx=None, defer=False):
    own = p is None
    if own:
        p = Prog()
    NK = int(np.log2(TC))
    NCH = LL // TC
    uT = p.D("uT", [nunits, 32, LL], BF16, "ExternalInput")
    prm = p.D("prm", [nunits, 128, 67], F32, "ExternalInput")
    dd = p.D("dd", [nunits, 32, 1], F32, "ExternalInput")
    yT = p.D("yT", [nunits, 32, LL], F32, "ExternalOutput")
    identf = ident_f32(p)
    pow2 = p.sb("pow2", [128, NK], F32)
    for k in range(NK):
        p.op("pool", lambda e, k=k: e.memset(pow2[:, k:k + 1], float(2 ** k)), writes=["pow2"])
    NS = 2
    ub = [p.sb(f"ub{i}", [32, LL], BF16) for i in range(NS)]
    P_ = [p.sb(f"prm{i}", [128, 67], F32) for i in range(NS)]
    dsb = [p.sb(f"dsb{i}", [32, 1], F32) for i in range(NS)]
    st_s = [{n: p.sb(f"{n}{i}", [128, TC], F32) for n in ("reA", "imA", "reB", "imB")} for i in range(NS)]
    rebf_s = [p.sb(f"rebf{i}", [128, TC], BF16) for i in range(NS)]
    rr_t_s = [p.sb(f"rr_t{i}", [128, NK], F32) for i in range(NS)]
    rr_i_s = [p.sb(f"rr_i{i}", [128, NK], mybir.dt.int32) for i in range(NS)]
    tmp2_s = [None] * NS
    dx_s = [p.sb(f"s5dx{i}", [32, 32], BF16) for i in range(NS)]
    imbf_s = [p.sb(f"imbf{i}", [128, TC], BF16) for i in range(NS)]
    p3bf_s = [p.sb(f"p3bf{i}", [128, TC], BF16) for i in range(NS)]
    p4bf_s = [p.sb(f"p4bf{i}", [128, TC], BF16) for i in range(NS)]
    zre_s = [p.sb(f"zre{i}", [128, TC], F32) for i in range(NS)]
    zim_s = [p.sb(f"zim{i}", [128, TC], F32) for i in range(NS)]
    cosT_s = [p.sb(f"cosT{i}", [128, TC], F32) for i in range(NS)]
    sinT_s = [p.sb(f"sinT{i}", [128, TC], F32) for i in range(NS)]
    rT_s = [p.sb(f"rT{i}", [128, TC], F32) for i in range(NS)]
    T1 = p.sb("s5T1", [128, TC], F32)
    rrT = p.sb("s5rrT", [128, TC], F32)
    rrI = p.sb("s5rrI", [128, TC], mybir.dt.int32)
    iota1 = p.sb("s5iota1", [128, TC], F32)
    assert TC == 512
    p.op("pool", lambda e: e.memset(T1[:], 0.5), writes=["T1"])
    p.op("dve", lambda e: e.tensor_tensor_scan(out=iota1[:], data0=T1[:], data1=T1[:], initial=0.0, op0=ALU.add, op1=ALU.add), reads=["T1"], writes=["iota1"])
    sm_s = [{n: p.sb(f"s5_{n}{i}", [128, NK], F32) for n in ("xk", "tk", "mag", "s1", "c1", "sin", "cos", "ar", "ai", "nai")} for i in range(NS)]
    col_s = [{n: p.sb(f"s5c_{n}{i}", [128, 1], F32) for n in ("dl", "x", "th", "m1", "den", "wre", "wim", "nwim", "t1", "t2", "cre", "cim", "t3", "t4")} for i in range(NS)]
    bb_s = [{n: p.sb(f"s5b_{n}{i}", [128, 16], F32) for n in ("bre", "bim")} for i in range(NS)]
    bx_s = [{n: p.sb(f"s5x_{n}{i}", [128, 32], F32) for n in ("bre", "bim")} for i in range(NS)]
    cxs_s = [{n: p.sb(f"s5cx_{n}{i}", [128, 32], BF16) for n in ("cre", "ncim", "ncre")} for i in range(NS)]
    lb_s = [{n: p.sb(f"s5l_{n}{i}", [32, 128], BF16) for n in ("bre", "bim")} for i in range(NS)]
    GLOBAL_KEYS = {"T1", "rrT", "rrI", "iota1", "pow2", "identf", "ps_t", "ps_bu0", "ps_bu1", "ps_bu2", "ps_bu3", "ps_y0", "ps_y1", "ysb0", "ysb1"}
    if psx is None:
        ps_t = p.ps("ps_t", [32, 128], F32)
        ps_bu = [p.ps(f"ps_bu{i}", [128, 512], F32) for i in range(4)]
        ps_y = [p.ps(f"ps_y{i}", [32, 512], F32) for i in range(2)]
    else:
        ps_t = psx[0:32, 0:128]
        ps_bu = [psx] * 4
        ps_y = [psx[0:32, :]] * 2
    ysb = [p.sb(f"ysb{i}", [32, 512], F32) for i in range(2)]

    def V(e, out, in0, s1, s2, op0, op1=None):
        if op1 is None:
            return e.tensor_scalar(out=out, in0=in0, scalar1=s1, scalar2=None, op0=op0)
        return e.tensor_scalar(out=out, in0=in0, scalar1=s1, scalar2=s2, op0=op0, op1=op1)

    iyc = [0]

    def unit_gen(u, b):
        def kk(k_):
            if psx is not None and k_.startswith("ps_"):
                return "psx"
            return k_ if k_ in GLOBAL_KEYS else f"s{b}_{k_}"

        def op(eng, fn, reads=(), writes=()):
            p.op(eng, fn, [kk(r) for r in reads], [kk(w) for w in writes])

        def dma(q, out, in_, semkey, reads=(), writes=(), **kw):
            p.dma(q, out, in_, semkey, [kk(r) for r in reads], [kk(w) for w in writes], **kw)

        st, rebf, imbf, tmp2, sm, col, bb, bx, cxs, lb_, rr_t, rr_i = (st_s[b], rebf_s[b], imbf_s[b], tmp2_s[b], sm_s[b], col_s[b], bb_s[b],
                                                                        bx_s[b], cxs_s[b], lb_s[b], rr_t_s[b], rr_i_s[b])
        dx = dx_s[b]
        p3bf, p4bf, zre, zim, cosT, sinT, rT = p3bf_s[b], p4bf_s[b], zre_s[b], zim_s[b], cosT_s[b], sinT_s[b], rT_s[b]
        dma("sp", ub[b][:], uT[u, :, :], f"ld_u{b}", writes=[f"ub{b}"])
        dma("sp", P_[b][:], prm[u, :, :], f"ld_p{b}", writes=[f"prm{b}"])
        dma("sp", dsb[b][:], dd[u, :, :], f"ld_d{b}", writes=[f"dsb{b}"])
        Pm = P_[b]
        pk = f"prm{b}"
        c = col
        op("act", lambda e, Pm=Pm: e.activation(out=c["dl"][:], in_=Pm[:, 2:3], func=AF.Exp), reads=[pk], writes=["c_dl"])
        op("dve", lambda e, Pm=Pm: e.tensor_tensor(out=c["x"][:], in0=Pm[:, 0:1], in1=c["dl"][:], op=ALU.mult), reads=[pk, "c_dl"], writes=["c_x"])
        op("dve", lambda e, Pm=Pm: e.tensor_tensor(out=c["th"][:], in0=Pm[:, 1:2], in1=c["dl"][:], op=ALU.mult), reads=[pk, "c_dl"], writes=["c_th"])
        op("dve", lambda e: V(e, sm["xk"][:], pow2[:], c["x"][:, 0:1], None, ALU.mult), reads=["pow2", "c_x"], writes=["xk"])
        op("dve", lambda e: V(e, sm["tk"][:], pow2[:], c["th"][:, 0:1], None, ALU.mult), reads=["pow2", "c_th"], writes=["tk"])
        op("act", lambda e: e.activation(out=sm["mag"][:], in_=sm["xk"][:], func=AF.Exp), reads=["xk"], writes=["mag"])
        for nm, shift in (("s1", 0.0), ("c1", 0.5 * PI)):
            R_ = sm[nm]
            op("dve", lambda e, R_=R_, shift=shift: V(e, R_[:], sm["tk"][:], shift, None, ALU.add), reads=["tk"], writes=[nm])
            op("dve", lambda e, R_=R_: V(e, rr_t[:], R_[:], 1.0 / (2 * PI), None, ALU.mult), reads=[nm], writes=["rr_t"])
            op("dve", lambda e: e.tensor_copy(out=rr_i[:], in_=rr_t[:]), reads=["rr_t"], writes=["rr_i"])
            op("dve", lambda e: e.tensor_copy(out=rr_t[:], in_=rr_i[:]), reads=["rr_i"], writes=["rr_t"])
            op("dve", lambda e, R_=R_: e.scalar_tensor_tensor(out=R_[:], in0=rr_t[:], scalar=-6.28125, in1=R_[:], op0=ALU.mult, op1=ALU.add),
                 reads=["rr_t", nm], writes=[nm])
            op("dve", lambda e, R_=R_: e.scalar_tensor_tensor(out=R_[:], in0=rr_t[:], scalar=-(2 * PI - 6.28125), in1=R_[:], op0=ALU.mult, op1=ALU.add),
                 reads=["rr_t", nm], writes=[nm])
            op("dve", lambda e, R_=R_: e.tensor_single_scalar(out=rr_t[:], in_=R_[:], scalar=PI, op=ALU.is_gt), reads=[nm], writes=["rr_t"])
            op("dve", lambda e, R_=R_: e.scalar_tensor_tensor(out=R_[:], in0=rr_t[:], scalar=-2 * PI, in1=R_[:], op0=ALU.mult, op1=ALU.add),
                 reads=["rr_t", nm], writes=[nm])
        op("act", lambda e: e.activation(out=sm["sin"][:], in_=sm["s1"][:], func=AF.Sin), reads=["s1"], writes=["sin"])
        op("act", lambda e: e.activation(out=sm["cos"][:], in_=sm["c1"][:], func=AF.Sin), reads=["c1"], writes=["cos"])
        op("dve", lambda e: e.tensor_tensor(out=sm["ar"][:], in0=sm["mag"][:], in1=sm["cos"][:], op=ALU.mult), reads=["mag", "cos"], writes=["ar"])
        op("dve", lambda e: e.tensor_tensor(out=sm["ai"][:], in0=sm["mag"][:], in1=sm["sin"][:], op=ALU.mult), reads=["mag", "sin"], writes=["ai"])
        op("dve", lambda e: V(e, sm["nai"][:], sm["ai"][:], -1.0, None, ALU.mult), reads=["ai"], writes=["nai"])
        op("dve", lambda e: V(e, c["m1"][:], sm["ar"][:, 0:1], -1.0, None, ALU.add), reads=["ar"], writes=["c_m1"])
        op("dve", lambda e, Pm=Pm: e.tensor_tensor(out=c["t1"][:], in0=Pm[:, 0:1], in1=Pm[:, 0:1], op=ALU.mult), reads=[pk], writes=["c_t1"])
        op("dve", lambda e, Pm=Pm: e.scalar_tensor_tensor(out=c["den"][:], in0=Pm[:, 1:2], scalar=Pm[:, 1:2], in1=c["t1"][:], op0=ALU.mult, op1=ALU.add),
             reads=[pk, "c_t1"], writes=["c_den"])
        op("dve", lambda e: e.reciprocal(out=c["den"][:], in_=c["den"][:]), reads=["c_den"], writes=["c_den"])
        op("dve", lambda e, Pm=Pm: e.tensor_tensor(out=c["t1"][:], in0=c["m1"][:], in1=Pm[:, 0:1], op=ALU.mult), reads=[pk, "c_m1", "c_den"], writes=["c_t1"])
        op("dve", lambda e, Pm=Pm: e.scalar_tensor_tensor(out=c["t1"][:], in0=sm["ai"][:, 0:1], scalar=Pm[:, 1:2], in1=c["t1"][:], op0=ALU.mult, op1=ALU.add),
             reads=[pk, "ai", "c_t1"], writes=["c_t1"])
        op("dve", lambda e: e.tensor_tensor(out=c["wre"][:], in0=c["t1"][:], in1=c["den"][:], op=ALU.mult), reads=["c_t1", "c_den"], writes=["c_wre"])
        op("dve", lambda e, Pm=Pm: e.tensor_tensor(out=c["t2"][:], in0=c["m1"][:], in1=Pm[:, 1:2], op=ALU.mult), reads=[pk, "c_m1"], writes=["c_t2"])
        op("dve", lambda e, Pm=Pm: e.scalar_tensor_tensor(out=c["t2"][:], in0=sm["ai"][:, 0:1], scalar=Pm[:, 0:1], in1=c["t2"][:], op0=ALU.mult, op1=ALU.subtract),
             reads=[pk, "ai", "c_t2"], writes=["c_t2"])
        op("dve", lambda e: e.tensor_tensor(out=c["wim"][:], in0=c["t2"][:], in1=c["den"][:], op=ALU.mult), reads=["c_t2", "c_den"], writes=["c_wim"])
        op("dve", lambda e: V(e, c["nwim"][:], c["wim"][:], -1.0, None, ALU.mult), reads=["c_wim"], writes=["c_nwim"])
        op("dve", lambda e, Pm=Pm: V(e, bb["bre"][:], Pm[:, 3:19], c["wre"][:, 0:1], None, ALU.mult), reads=[pk, "c_wre"], writes=["b_bre"])
        op("dve", lambda e, Pm=Pm: e.scalar_tensor_tensor(out=bb["bre"][:], in0=Pm[:, 19:35], scalar=c["nwim"][:, 0:1], in1=bb["bre"][:], op0=ALU.mult, op1=ALU.add),
             reads=[pk, "c_nwim", "b_bre"], writes=["b_bre"])
        op("dve", lambda e, Pm=Pm: V(e, bb["bim"][:], Pm[:, 19:35], c["wre"][:, 0:1], None, ALU.mult), reads=[pk, "c_wre"], writes=["b_bim"])
        op("dve", lambda e, Pm=Pm: e.scalar_tensor_tensor(out=bb["bim"][:], in0=Pm[:, 3:19], scalar=c["wim"][:, 0:1], in1=bb["bim"][:], op0=ALU.mult, op1=ALU.add),
             reads=[pk, "c_wim", "b_bim"], writes=["b_bim"])
        for n in ("bre", "bim"):
            op("pool", lambda e, n=n: e.memset(bx[n][:], 0.0), writes=["x_" + n])
            for g2 in range(2):
                op("dve", lambda e, n=n, g2=g2: e.tensor_copy(out=bx[n][g2 * 64:(g2 + 1) * 64, g2 * 16:(g2 + 1) * 16], in_=bb[n][g2 * 64:(g2 + 1) * 64, :]),
                     reads=["b_" + n, "x_" + n], writes=["x_" + n])
            op("pe", lambda e, n=n: e.transpose(out=ps_t[:], in_=bx[n][:], identity=identf[:]), reads=["x_" + n, "identf"], writes=["ps_t"])
            op("dve", lambda e, n=n: e.tensor_copy(out=lb_[n][:], in_=ps_t[:]), reads=["ps_t"], writes=["l_" + n])
        for n, c0, sgn in (("cre", 35, 1.0), ("ncim", 51, -1.0), ("ncre", 35, -1.0)):
            op("pool", lambda e, n=n: e.memset(cxs[n][:], 0.0), writes=["cx_" + n])
            for g2 in range(2):
                op("dve", lambda e, n=n, g2=g2, c0=c0, sgn=sgn, Pm=Pm: V(e, cxs[n][g2 * 64:(g2 + 1) * 64, g2 * 16:(g2 + 1) * 16],
                                                                         Pm[g2 * 64:(g2 + 1) * 64, c0:c0 + 16], sgn, None, ALU.mult),
                     reads=[pk, "cx_" + n], writes=["cx_" + n])
        op("dve", lambda e: e.tensor_scalar(out=dx[:], in0=identf[0:32, 0:32], scalar1=dsb[b][:, 0:1], scalar2=None, op0=ALU.mult),
           reads=["identf", f"dsb{b}"], writes=["dx"])
        yield
        for nm, shift, R_ in (("sinT", 0.0, sinT), ("cosT", 0.5 * PI, cosT)):
            op("dve", lambda e, R_=R_, shift=shift: V(e, R_[:], iota1[:], c["th"][:, 0:1], shift, ALU.mult, ALU.add), reads=["iota1", "c_th"], writes=[nm])
            op("dve", lambda e, R_=R_: V(e, rrT[:], R_[:], 1.0 / (2 * PI), None, ALU.mult), reads=[nm], writes=["rrT"])
            op("dve", lambda e: e.tensor_copy(out=rrI[:], in_=rrT[:]), reads=["rrT"], writes=["rrI"])
            op("dve", lambda e: e.tensor_copy(out=rrT[:], in_=rrI[:]), reads=["rrI"], writes=["rrT"])
            op("dve", lambda e, R_=R_: e.scalar_tensor_tensor(out=R_[:], in0=rrT[:], scalar=-6.28125, in1=R_[:], op0=ALU.mult, op1=ALU.add),
               reads=["rrT", nm], writes=[nm])
            op("dve", lambda e, R_=R_: e.scalar_tensor_tensor(out=R_[:], in0=rrT[:], scalar=-(2 * PI - 6.28125), in1=R_[:], op0=ALU.mult, op1=ALU.add),
               reads=["rrT", nm], writes=[nm])
            yield
            op("dve", lambda e, R_=R_: e.tensor_single_scalar(out=rrT[:], in_=R_[:], scalar=PI, op=ALU.is_gt), reads=[nm], writes=["rrT"])
            op("dve", lambda e, R_=R_: e.scalar_tensor_tensor(out=R_[:], in0=rrT[:], scalar=-2 * PI, in1=R_[:], op0=ALU.mult, op1=ALU.add),
               reads=["rrT", nm], writes=[nm])
            op("dve", lambda e, R_=R_: e.tensor_single_scalar(out=rrT[:], in_=R_[:], scalar=-PI, op=ALU.is_lt), reads=[nm], writes=["rrT"])
            op("dve", lambda e, R_=R_: e.scalar_tensor_tensor(out=R_[:], in0=rrT[:], scalar=2 * PI, in1=R_[:], op0=ALU.mult, op1=ALU.add),
               reads=["rrT", nm], writes=[nm])
            op("act", lambda e, R_=R_: e.activation(out=R_[:], in_=R_[:], func=AF.Sin), reads=[nm], writes=[nm])
            yield
        op("dve", lambda e: V(e, rT[:], iota1[:], 0.0, sm["mag"][:, 0:1], ALU.mult, ALU.add), reads=["iota1", "mag"], writes=["rT"])
        TT = lambda e, o, a, b_, o_=ALU.mult: e.tensor_tensor(out=o, in0=a, in1=b_, op=o_)
        for ch in range(NCH):
            t0 = ch * TC
            for n_i, (n, dst) in enumerate((("bre", "reA"), ("bim", "imA"))):
                pb = n_i
                op("pe", lambda e, pb=pb, n=n, b=b, t0=t0: e.matmul(ps_bu[pb][:], lhsT=lb_[n][:], rhs=ub[b][:, t0:t0 + TC], start=True, stop=True),
                   reads=["l_" + n, f"ub{b}"], writes=[f"ps_bu{pb}"])
                op("dve", lambda e, pb=pb, dst=dst: e.tensor_copy(out=st[dst][:], in_=ps_bu[pb][:]), reads=[f"ps_bu{pb}"], writes=[dst])
                if n_i == 0:
                    yield
            op("dve", lambda e: TT(e, st["reB"][:], cosT[:], st["reA"][:]), reads=["cosT", "reA"], writes=["reB"])
            op("dve", lambda e: TT(e, T1[:], sinT[:], st["imA"][:]), reads=["sinT", "imA"], writes=["T1"])
            op("dve", lambda e: TT(e, st["reB"][:], st["reB"][:], T1[:], ALU.add), reads=["reB", "T1"], writes=["reB"])
            yield
            op("dve", lambda e: TT(e, T1[:], sinT[:], st["reA"][:]), reads=["sinT", "reA"], writes=["T1"])
            op("dve", lambda e: TT(e, st["imB"][:], cosT[:], st["imA"][:]), reads=["cosT", "imA"], writes=["imB"])
            op("dve", lambda e: TT(e, st["imB"][:], st["imB"][:], T1[:], ALU.subtract), reads=["imB", "T1"], writes=["imB"])
            ini_r = c["cre"][:] if ch > 0 else 0.0
            ini_i = c["cim"][:] if ch > 0 else 0.0
            op("dve", lambda e, ini_r=ini_r: e.tensor_tensor_scan(out=zre[:], data0=rT[:], data1=st["reB"][:], initial=ini_r, op0=ALU.mult, op1=ALU.add),
               reads=["rT", "reB", "c_cre"], writes=["zre"])
            op("dve", lambda e, ini_i=ini_i: e.tensor_tensor_scan(out=zim[:], data0=rT[:], data1=st["imB"][:], initial=ini_i, op0=ALU.mult, op1=ALU.add),
               reads=["rT", "imB", "c_cim"], writes=["zim"])
            if ch + 1 < NCH:
                cl, sl = cosT[:, TC - 1:TC], sinT[:, TC - 1:TC]
                zr, zi = zre[:, TC - 1:TC], zim[:, TC - 1:TC]
                op("dve", lambda e, zi=zi, sl=sl: TT(e, c["t3"][:], zi, sl), reads=["zim", "sinT"], writes=["c_t3"])
                op("dve", lambda e, zr=zr, cl=cl: e.scalar_tensor_tensor(out=c["cre"][:], in0=zr, scalar=cl, in1=c["t3"][:], op0=ALU.mult, op1=ALU.subtract),
                   reads=["zre", "cosT", "c_t3"], writes=["c_cre"])
                op("dve", lambda e, zr=zr, sl=sl: TT(e, c["t4"][:], zr, sl), reads=["zre", "sinT"], writes=["c_t4"])
                op("dve", lambda e, zi=zi, cl=cl: e.scalar_tensor_tensor(out=c["cim"][:], in0=zi, scalar=cl, in1=c["t4"][:], op0=ALU.mult, op1=ALU.add),
                   reads=["zim", "cosT", "c_t4"], writes=["c_cim"])
            yield
            op("dve", lambda e: TT(e, st["reB"][:], cosT[:], zre[:]), reads=["cosT", "zre"], writes=["reB"])
            op("dve", lambda e: TT(e, T1[:], sinT[:], zim[:]), reads=["sinT", "zim"], writes=["T1"])
            op("dve", lambda e: TT(e, rebf[:], st["reB"][:], T1[:], ALU.subtract), reads=["reB", "T1"], writes=["rebf"])
            yield
            op("dve", lambda e: TT(e, st["imB"][:], sinT[:], zre[:]), reads=["sinT", "zre"], writes=["imB"])
            op("dve", lambda e: TT(e, T1[:], cosT[:], zim[:]), reads=["cosT", "zim"], writes=["T1"])
            op("dve", lambda e: TT(e, imbf[:], st["imB"][:], T1[:], ALU.add), reads=["imB", "T1"], writes=["imbf"])
            yield
            yi = iyc[0] % 2
            iyc[0] += 1
            for mi, (wn, rhs_, rk) in enumerate((("cre", rebf, "rebf"), ("ncim", imbf, "imbf"))):
                op("pe", lambda e, yi=yi, wn=wn, rhs_=rhs_, mi=mi: e.matmul(ps_y[yi][:], lhsT=cxs[wn][:], rhs=rhs_[:], start=(mi == 0), stop=False),
                   reads=["cx_" + wn, rk], writes=[f"ps_y{yi}"])
            op("pe", lambda e, yi=yi, b=b, t0=t0: e.matmul(ps_y[yi][:], lhsT=dx[:], rhs=ub[b][:, t0:t0 + TC], start=False, stop=True),
               reads=["dx", f"ub{b}"], writes=[f"ps_y{yi}"])
            op("dve", lambda e, yi=yi: e.tensor_copy(out=ysb[yi][:], in_=ps_y[yi][:]), reads=[f"ps_y{yi}"], writes=[f"ysb{yi}"])
            dma("sp", yT[u, :, t0:t0 + TC], ysb[yi][:], f"st_y{yi}", reads=[f"ysb{yi}"])
            yield

    def slot_gen(b):
        for u in range(b, nunits, NS):
            yield from unit_gen(u, b)

    gens = [slot_gen(b) for b in range(min(NS, nunits))]
    if defer:
        return gens
    while gens:
        for g in list(gens):
            try:
                next(g)
            except StopIteration:
                gens.remove(g)
    return p.build() if own else p.phase_end()


DFF = 2816
GC = 1.5957691216057308


def wview(w):
    return w.rearrange("(c p) n -> p c n", p=128)


def emit_ffn(p, hT, hkey, xres, NT, experts, pm, wbufs, actTs, sgt, cw=None, after_final=None, side=None):
    NTC = NT // 4
    NA = len(actTs)
    blocks = []
    for ei, (wg, wu, wd, F) in enumerate(experts):
        for fb in range(F // 256):
            blocks.append((ei, wg, wu, wd, fb))

    def down_units(k):
        ei, wg, wu, wd, fb = blocks[k]
        j = k % 2
        wdb = wbufs[j][2]
        actT = actTs[k % NA]
        ak = f"actT{k % NA}"
        units = []
        for i in range(NT):
            for dh in range(2):
                def emit(i=i, dh=dh):
                    di = 4 + (i * 2 + dh) % 2
                    pd = pm[di]
                    for fc in range(2):
                        p.op("pe", lambda e, pd=pd, fc=fc: e.matmul(pd[:], lhsT=actT[:, fc, i * 128:(i + 1) * 128], rhs=wdb[:, fc, dh * 512:(dh + 1) * 512],
                                                                   start=(fc == 0), stop=(fc == 1)),
                             reads=[f"wd{j}", f"{ak}_{fc}_{i // 4}"], writes=[f"pm{di}"])
                    xs = xres[:, i, dh * 512:(dh + 1) * 512]
                    if cw is None:
                        p.op("dve", lambda e, pd=pd, xs=xs: e.tensor_tensor(out=xs, in0=pd[:], in1=xs, op=ALU.add),
                             reads=[f"pm{di}", f"xres{i}"], writes=[f"xres{i}"])
                    else:
                        p.op("dve", lambda e, pd=pd, xs=xs: e.scalar_tensor_tensor(out=xs, in0=pd[:], scalar=cw[:, i, ei:ei + 1], in1=xs, op0=ALU.mult, op1=ALU.add),
                             reads=[f"pm{di}", f"xres{i}", f"cw{i}"], writes=[f"xres{i}"])
                units.append(emit)
        return units

    def flush_units(units, final):
        for idx, fn in enumerate(units):
            fn()
            if final and after_final is not None and idx % 2 == 1 and idx // 2 >= 1:
                after_final(idx // 2 - 1)
        if final and after_final is not None and units:
            after_final(NT - 1)

    pending = []
    for k, (ei, wg, wu, wd, fb) in enumerate(blocks):
        j = k % 2
        wgb, wub, wdb = wbufs[j]
        actT = actTs[k % NA]
        ak = f"actT{k % NA}"
        wgv, wuv = wview(wg), wview(wu)
        wdv = wd.rearrange("(c p) n -> p c n", p=128)
        p.dma("pool", wgb[:], wgv[:, :, fb * 256:(fb + 1) * 256], f"ld_wg{j}", writes=[f"wg{j}"])
        p.dma("pool", wub[:], wuv[:, :, fb * 256:(fb + 1) * 256], f"ld_wu{j}", writes=[f"wu{j}"])
        p.dma("pool", wdb[:], wdv[:, fb * 2:fb * 2 + 2, :], f"ld_wd{j}", writes=[f"wd{j}"])
        nun = 2 * NTC
        per = (len(pending) + nun - 1) // nun if pending else 0
        ui = 0
        for fc in range(2):
            for tc in range(NTC):
                gi = (fc * NTC + tc) % 2
                pg, pu = pm[gi], pm[2 + gi]
                ts_ = slice(tc * 512, (tc + 1) * 512)
                for kc in range(8):
                    p.op("pe", lambda e, pg=pg, kc=kc, fc=fc, ts_=ts_, wgb=wgb: e.matmul(pg[:], lhsT=wgb[:, kc, fc * 128:(fc + 1) * 128], rhs=hT[:, kc, ts_],
                                                                                     start=(kc == 0), stop=(kc == 7)),
                         reads=[f"wg{j}", f"{hkey}_{kc}_{tc}"], writes=[f"pm{gi}"])
                for kc in range(8):
                    p.op("pe", lambda e, pu=pu, kc=kc, fc=fc, ts_=ts_, wub=wub: e.matmul(pu[:], lhsT=wub[:, kc, fc * 128:(fc + 1) * 128], rhs=hT[:, kc, ts_],
                                                                                     start=(kc == 0), stop=(kc == 7)),
                         reads=[f"wu{j}", f"{hkey}_{kc}_{tc}"], writes=[f"pm{2 + gi}"])
                p.op("act", lambda e, pg=pg, gi=gi: e.activation(out=sgt[gi][:], in_=pg[:], func=AF.Silu), reads=[f"pm{gi}"], writes=[f"sgt{gi}"])
                p.op("dve", lambda e, pu=pu, gi=gi, fc=fc, ts_=ts_, actT=actT: e.tensor_tensor(out=actT[:, fc, ts_], in0=sgt[gi][:], in1=pu[:], op=ALU.mult),
                     reads=[f"pm{2 + gi}", f"sgt{gi}"], writes=[f"{ak}_{fc}_{tc}"])
                for fn in pending[ui * per:(ui + 1) * per]:
                    fn()
                ui += 1
                if side is not None:
                    next(side, None)
                    next(side, None)
        if side is not None:
            for _ in side:
                pass
            side = None
        for fn in pending[ui * per:]:
            fn()
        pending = down_units(k)
        if NA == 1:
            flush_units(pending, k == len(blocks) - 1)
            pending = []
    flush_units(pending, True)


def build_phase_c(NT=TPC // 128, nsub=1, p=None):
    own = p is None
    if own:
        p = Prog()
    T = NT * 128
    NTC = NT // 4
    x = p.D("x", [nsub * T, D], F32, "ExternalInput")
    ysT = p.D("ysT", [512, nsub * T], F32, "ExternalInput")
    sbT = p.D("sbT", [512, nsub * T], BF16, "ExternalInput")
    wglu = p.D("wglu", [512, 512], F32, "ExternalInput")
    bglu = p.D("bglu", [128, 4], F32, "ExternalInput")
    wout = p.D("wout", [D, D], F32, "ExternalInput")
    g_ffn = p.D("g_ffn", [128, 8], F32, "ExternalInput")
    wg = p.D("wg", [D, DFF], F32, "ExternalInput")
    wu = p.D("wu", [D, DFF], F32, "ExternalInput")
    wd = p.D("wd", [DFF, D], F32, "ExternalInput")
    g_mix = p.D("g_mix", [128, 8], F32, "ExternalInput")
    win = p.D("win", [D, 3088], F32, "ExternalInput")
    x1 = p.D("x1", [T, D], F32, "ExternalOutput")
    featT = p.D("featT", [2048, nsub * T], BF16, "ExternalOutput")
    v1 = p.D("v1", [nsub * T, 1024], BF16, "ExternalOutput")
    flT = p.D("flT", [16, nsub * T], F32, "ExternalOutput")

    ident = ident_bf16(p)
    sc = norm_scratch(p, with_xt=False)
    xres = p.sb("xres", [128, NT, D], F32)
    bigA = p.sb("bigA", [128, 8, T], BF16)
    bigB = p.sb("bigB", [128, 8, T], BF16)
    pm = [p.ps(f"pm{i}", [128, 512], F32) for i in range(6)]
    memo = {}

    def SB(name, shape, dt):
        if name not in memo:
            memo[name] = p.sb(name, shape, dt)
        return memo[name]

    def PS(name, shape, dt=F32):
        if name not in memo:
            memo[name] = p.ps(name, shape, dt)
        return memo[name]

    for sub in range(nsub):
        T0 = sub * T
        gains = SB("gains", [128, 16], F32)
        bgl = SB("bgl", [128, 4], F32)
        p.dma("sp", gains[:, 0:8], g_ffn[:, :], "ld_g0", writes=["gain0"])
        p.dma("sp", gains[:, 8:16], g_mix[:, :], "ld_g1", writes=["gain1"])
        p.dma("sp", bgl[:], bglu[:, :], "ld_bg", writes=["bgl"])
        wglu_sb = SB("wglu_sb", [128, 4, 512], BF16)
        p.dma("pool", wglu_sb[:], wview(wglu), "ld_wglu", writes=["wglu"])
        wout_sb = SB("wout_sb", [128, 8, 1024], BF16)
        for h in range(2):
            p.dma("pool", wout_sb[:, :, h * 512:(h + 1) * 512], wview(wout)[:, :, h * 512:(h + 1) * 512], f"ld_wo{h}", writes=[f"wout{h}"])
        for i in range(NT):
            p.dma("sp", xres[:, i, :], x[T0 + i * 128:T0 + (i + 1) * 128, :], f"ld_x{i}", writes=[f"xres{i}"])
        for hc in range(4):
            p.dma("sp", bigA[:, 4 + hc, :], sbT[hc * 128:(hc + 1) * 128, T0:T0 + T], f"ld_sb{hc}", writes=[f"bigA_{4 + hc}_{tc}" for tc in range(NTC)])
        yst = [SB(f"yst{i}", [128, 512], F32) for i in range(2)]
        gt = [SB(f"gt{i}", [128, 512], F32) for i in range(2)]
        sgt = [SB(f"sgt{i}", [128, 512], F32) for i in range(2)]
        k = 0
        for tc in range(NTC):
            ts_ = slice(tc * 512, (tc + 1) * 512)
            for cc in range(4):
                b = k % 2
                k += 1
                p.dma("sp", yst[b][:], ysT[cc * 128:(cc + 1) * 128, T0 + tc * 512:T0 + (tc + 1) * 512], f"ld_y{b}", writes=[f"yst{b}"])
                p.op("dve", lambda e, b=b: e.tensor_tensor(out=gt[b][:], in0=yst[b][:], in1=yst[b][:], op=ALU.mult), reads=[f"yst{b}"], writes=[f"gt{b}"])
                p.op("dve", lambda e, b=b: e.tensor_scalar(out=gt[b][:], in0=gt[b][:], scalar1=0.044715, scalar2=1.0, op0=ALU.mult, op1=ALU.add), reads=[f"gt{b}"], writes=[f"gt{b}"])
                p.op("dve", lambda e, b=b: e.tensor_tensor(out=gt[b][:], in0=gt[b][:], in1=yst[b][:], op=ALU.mult), reads=[f"gt{b}", f"yst{b}"], writes=[f"gt{b}"])
                p.op("act", lambda e, b=b: e.activation(out=gt[b][:], in_=gt[b][:], func=AF.Sigmoid, scale=GC), reads=[f"gt{b}"], writes=[f"gt{b}"])
                p.op("dve", lambda e, b=b, cc=cc, ts_=ts_: e.tensor_tensor(out=bigB[:, cc, ts_], in0=gt[b][:], in1=yst[b][:], op=ALU.mult),
                     reads=[f"gt{b}", f"yst{b}"], writes=[f"bigB_{cc}_{tc}"])
            for n in range(4):
                gi = n % 2
                for cc in range(4):
                    p.op("pe", lambda e, gi=gi, cc=cc, n=n, ts_=ts_: e.matmul(pm[gi][:], lhsT=wglu_sb[:, cc, n * 128:(n + 1) * 128], rhs=bigB[:, cc, ts_],
                                                                          start=(cc == 0), stop=(cc == 3)),
                         reads=["wglu", f"bigB_{cc}_{tc}"], writes=[f"pm{gi}"])
                p.op("act", lambda e, gi=gi, n=n: e.activation(out=sgt[gi][:], in_=pm[gi][:], func=AF.Sigmoid, bias=bgl[:, n:n + 1]),
                     reads=[f"pm{gi}", "bgl"], writes=[f"sgt{gi}"])
                p.op("dve", lambda e, gi=gi, n=n, ts_=ts_: e.tensor_tensor(out=bigA[:, n, ts_], in0=bigB[:, n, ts_], in1=sgt[gi][:], op=ALU.mult),
                     reads=[f"sgt{gi}", f"bigB_{n}_{tc}"], writes=[f"bigA_{n}_{tc}"])
        for i in range(NT):
            for dh in range(2):
                di = 2 + (i * 2 + dh) % 2
                for kc in range(8):
                    p.op("pe", lambda e, di=di, kc=kc, i=i, dh=dh: e.matmul(pm[di][:], lhsT=bigA[:, kc, i * 128:(i + 1) * 128], rhs=wout_sb[:, kc, dh * 512:(dh + 1) * 512],
                                                                        start=(kc == 0), stop=(kc == 7)),
                         reads=[f"wout{dh}", f"bigA_{kc}_{i // 4}"], writes=[f"pm{di}"])
                xs = xres[:, i, dh * 512:(dh + 1) * 512]
                p.op("dve", lambda e, di=di, xs=xs: e.tensor_tensor(out=xs, in0=pm[di][:], in1=xs, op=ALU.add), reads=[f"pm{di}", f"xres{i}"], writes=[f"xres{i}"])
            if i >= 1:
                emit_norm_transpose(p, None, gains[:, 0:8], "gain0", bigB, "bigB", ident, NT, sc, xres=xres, tiles=[i - 1])
        emit_norm_transpose(p, None, gains[:, 0:8], "gain0", bigB, "bigB", ident, NT, sc, xres=xres, tiles=[NT - 1])
        wbufs = [(SB(f"wgb{j}", [128, 8, 256], BF16), SB(f"wub{j}", [128, 8, 256], BF16), SB(f"wdb{j}", [128, 2, 1024], BF16)) for j in range(2)]
        actT = SB("actT", [128, 2, T], BF16)
        emit_ffn(p, bigB, "bigB", xres, NT, [(wg, wu, wd, DFF)], pm, wbufs, [actT], sgt,
                 after_final=lambda i: emit_norm_transpose(p, None, gains[:, 8:16], "gain1", bigA, "bigA", ident, NT, sc, xres=xres, tiles=[i]))
        for i in range(NT if sub == nsub - 1 else 0):
            p.dma("sp", x1[i * 128:(i + 1) * 128, :], xres[:, i, :], f"st_x{i % 4}", reads=[f"xres{i}"])
        winv = wview(win)
        wf = SB("wf", [128, 8, 16], BF16)
        p.dma("pool", wf[:], winv[:, :, 3072:3088], "ld_wf", writes=["wf"])
        ob = [SB(f"ob{i}", [128, 512], BF16) for i in range(4)]
        k = 0
        for blk in range(6):
            h = blk % 2
            w1 = wout_sb[:, :, h * 512:(h + 1) * 512]
            p.dma("pool", w1, winv[:, :, blk * 512:(blk + 1) * 512], f"ld_wo{h}", writes=[f"wout{h}"])
            if blk < 4:
                sc_ = 0.125 if blk < 2 else 1.0
                for n in range(4):
                    for tc in range(NTC):
                        pi_ = k % 4
                        k += 1
                        ts_ = slice(tc * 512, (tc + 1) * 512)
                        for kc in range(8):
                            p.op("pe", lambda e, pi_=pi_, kc=kc, n=n, ts_=ts_, w1=w1: e.matmul(pm[pi_][:], lhsT=w1[:, kc, n * 128:(n + 1) * 128], rhs=bigA[:, kc, ts_],
                                                                                           start=(kc == 0), stop=(kc == 7)),
                                 reads=[f"wout{h}", f"bigA_{kc}_{tc}"], writes=[f"pm{pi_}"])
                        if pi_ < 2:
                            p.op("act", lambda e, pi_=pi_, sc_=sc_: e.activation(out=ob[pi_][:], in_=pm[pi_][:], func=AF.Copy, scale=sc_), reads=[f"pm{pi_}"], writes=[f"ob{pi_}"])
                        else:
                            p.op("dve", lambda e, pi_=pi_, sc_=sc_: e.tensor_scalar(out=ob[pi_][:], in0=pm[pi_][:], scalar1=sc_, scalar2=None, op0=ALU.mult), reads=[f"pm{pi_}"], writes=[f"ob{pi_}"])
                        r0 = blk * 512 + n * 128
                        p.dma("sp", featT[r0:r0 + 128, T0 + tc * 512:T0 + (tc + 1) * 512], ob[pi_][:], f"st_ob{pi_}", reads=[f"ob{pi_}"])
            else:
                for i in range(NT):
                    pi_ = k % 4
                    k += 1
                    for kc in range(8):
                        p.op("pe", lambda e, pi_=pi_, kc=kc, i=i, w1=w1: e.matmul(pm[pi_][:], lhsT=bigA[:, kc, i * 128:(i + 1) * 128], rhs=w1[:, kc, :],
                                                                              start=(kc == 0), stop=(kc == 7)),
                             reads=[f"wout{h}", f"bigA_{kc}_{i // 4}"], writes=[f"pm{pi_}"])
                    if pi_ < 2:
                        p.op("act", lambda e, pi_=pi_: e.activation(out=ob[pi_][:], in_=pm[pi_][:], func=AF.Copy), reads=[f"pm{pi_}"], writes=[f"ob{pi_}"])
                    else:
                        p.op("dve", lambda e, pi_=pi_: e.tensor_copy(out=ob[pi_][:], in_=pm[pi_][:]), reads=[f"pm{pi_}"], writes=[f"ob{pi_}"])
                    p.dma("sp", v1[T0 + i * 128:T0 + (i + 1) * 128, (blk - 4) * 512:(blk - 3) * 512], ob[pi_][:], f"st_ob{pi_}", reads=[f"ob{pi_}"])
        for tc in range(NTC):
            ts_ = slice(tc * 512, (tc + 1) * 512)
            pi_ = 4 + tc % 2
            for kc in range(8):
                p.op("pe", lambda e, pi_=pi_, kc=kc, ts_=ts_: e.matmul(pm[pi_][0:16, :], lhsT=wf[:, kc, :], rhs=bigA[:, kc, ts_], start=(kc == 0), stop=(kc == 7)),
                     reads=["wf", f"bigA_{kc}_{tc}"], writes=[f"pm{pi_}"])
            p.op("dve", lambda e, pi_=pi_, tc=tc: e.tensor_copy(out=sgt[tc % 2][0:16, :], in_=pm[pi_][0:16, :]), reads=[f"pm{pi_}"], writes=[f"sgt{tc % 2}"])
            p.dma("sp", flT[:, T0 + tc * 512:T0 + (tc + 1) * 512], sgt[tc % 2][0:16, :], f"st_obf{tc % 2}", reads=[f"sgt{tc % 2}"])
    return p.build() if own else p.phase_end()


DFE = 3584
NEXP = 8


def build_phase_e(NT=TPC // 128, nexp=NEXP, dfe=DFE, p=None):
    own = p is None
    if own:
        p = Prog()
    T = NT * 128
    x1 = p.D("x1", [T, D], F32, "ExternalInput")
    coT = p.D("coT", [D, T], BF16, "ExternalInput")
    wout = p.D("wout", [D, D], F32, "ExternalInput")
    g_ffn = p.D("g_ffn", [128, 8], F32, "ExternalInput")
    g_row = p.D("g_row", [1, D], F32, "ExternalInput")
    wrT = p.D("wrT", [nexp, D], F32, "ExternalInput")
    mg = p.D("mg", [nexp, D, dfe], F32, "ExternalInput")
    mu = p.D("mu", [nexp, D, dfe], F32, "ExternalInput")
    md = p.D("md", [nexp, dfe, D], F32, "ExternalInput")
    gfin = p.D("gfin", [1, D], F32, "ExternalInput")
    out = p.D("out", [T, D], F32, "ExternalOutput")

    ident = ident_bf16(p)
    sc = norm_scratch(p, with_xt=False)
    xres = p.sb("xres", [128, NT, D], F32)
    bigA = p.sb("bigA", [128, 8, T], BF16)
    pm = [p.ps(f"pm{i}", [128, 512], F32) for i in range(6)]
    gains = p.sb("gains", [128, 8], F32)
    p.dma("sp", gains[:], g_ffn[:, :], "ld_g0", writes=["gain0"])
    gfb = p.sb("gfb", [128, D], F32)
    p.dma("sp", gfb[:], gfin.partition_broadcast(128), "ld_gf", writes=["gfb"])
    grb = p.sb("grb", [128, D], F32)
    p.dma("sp", grb[:], g_row.partition_broadcast(128), "ld_gr", writes=["grb"])
    wrb = p.sb("wrb", [128, nexp, D], F32)
    for e_ in range(nexp):
        p.dma("sp", wrb[:, e_, :], wrT[e_:e_ + 1, :].partition_broadcast(128), f"ld_wr{e_}", writes=[f"wrb{e_}"])
        p.op("pool", lambda e, e_=e_: e.tensor_tensor(out=wrb[:, e_, :], in0=wrb[:, e_, :], in1=grb[:], op=ALU.mult),
             reads=[f"wrb{e_}", "grb"], writes=[f"wrb{e_}"])
    wout_sb = p.sb("wout_sb", [128, 8, 1024], BF16)
    for h in range(2):
        p.dma("pool", wout_sb[:, :, h * 512:(h + 1) * 512], wview(wout)[:, :, h * 512:(h + 1) * 512], f"ld_wo{h}", writes=[f"wout{h}"])
    for i in range(NT):
        p.dma("sp", xres[:, i, :], x1[i * 128:(i + 1) * 128, :], f"ld_x{i}", writes=[f"xres{i}"])
    for kc in range(8):
        p.dma("sp", bigA[:, kc, :], coT[kc * 128:(kc + 1) * 128, :], f"ld_co{kc}", writes=[f"bigA_{kc}_{tc}" for tc in range(NT // 4)])
    for i in range(NT):
        for dh in range(2):
            di = 2 + (i * 2 + dh) % 2
            for kc in range(8):
                p.op("pe", lambda e, di=di, kc=kc, i=i, dh=dh: e.matmul(pm[di][:], lhsT=bigA[:, kc, i * 128:(i + 1) * 128], rhs=wout_sb[:, kc, dh * 512:(dh + 1) * 512],
                                                                    start=(kc == 0), stop=(kc == 7)),
                     reads=[f"wout{dh}", f"bigA_{kc}_{i // 4}"], writes=[f"pm{di}"])
            xs = xres[:, i, dh * 512:(dh + 1) * 512]
            p.op("dve", lambda e, di=di, xs=xs: e.tensor_tensor(out=xs, in0=pm[di][:], in1=xs, op=ALU.add), reads=[f"pm{di}", f"xres{i}"], writes=[f"xres{i}"])
    cw = p.sb("cw", [128, NT, nexp], F32)
    lg = p.sb("lg", [128, nexp], F32)
    mx = p.sb("mx", [128, 8], F32)
    msk = p.sb("msk", [128, nexp], F32)
    ex = p.sb("ex", [128, nexp], F32)
    nv1 = p.sb("nv1", [128, 1], F32)
    den = p.sb("den", [128, 1], F32)
    rj = p.sb("rj", [128, D], F32)
    ss, rs, junk, epsb = sc["ss"], sc["rs"], sc["junk"], sc["epsb"]
    tag = sc["tag"]
    def router_gen():
        for i in range(NT):
            xi = xres[:, i, :]
            p.op("act", lambda e, xi=xi: e.activation(out=junk[:], in_=xi, func=AF.Square, accum_out=ss[0][:]), reads=[f"xres{i}"], writes=[f"{tag}_junk", f"{tag}_ss0"])
            p.op("act", lambda e: e.activation(out=rs[0][:], in_=ss[0][:], func=AF.Sqrt, scale=1.0 / D, bias=epsb[:, 0:1]), reads=[f"{tag}_ss0", "epsb"], writes=[f"{tag}_rs0"])
            p.op("dve", lambda e: e.reciprocal(out=rs[0][:], in_=rs[0][:]), reads=[f"{tag}_rs0"], writes=[f"{tag}_rs0"])
            for e_ in range(nexp):
                p.op("dve", lambda e, xi=xi, e_=e_: e.scalar_tensor_tensor(out=rj[:], in0=xi, scalar=rs[0][:, 0:1], in1=wrb[:, e_, :], op0=ALU.mult, op1=ALU.mult,
                                                                        accum_out=lg[:, e_:e_ + 1]),
                     reads=[f"xres{i}", f"{tag}_rs0", f"wrb{e_}"], writes=["rj", "lg"])
            p.op("dve", lambda e: e.max(out=mx[:], in_=lg[:]), reads=["lg"], writes=["mx"])
            p.op("dve", lambda e: e.tensor_scalar(out=msk[:], in0=lg[:], scalar1=mx[:, 1:2], scalar2=None, op0=ALU.is_ge), reads=["lg", "mx"], writes=["msk"])
            p.op("dve", lambda e: e.tensor_scalar(out=nv1[:], in0=mx[:, 0:1], scalar1=-1.0, scalar2=None, op0=ALU.mult), reads=["mx"], writes=["nv1"])
            p.op("act", lambda e: e.activation(out=ex[:], in_=lg[:], func=AF.Exp, bias=nv1[:, 0:1]), reads=["lg", "nv1"], writes=["ex"])
            p.op("dve", lambda e: e.tensor_tensor(out=ex[:], in0=ex[:], in1=msk[:], op=ALU.mult), reads=["ex", "msk"], writes=["ex"])
            p.op("dve", lambda e: e.reduce_sum(out=den[:], in_=ex[:], axis=AX.X), reads=["ex"], writes=["den"])
            p.op("dve", lambda e: e.reciprocal(out=den[:], in_=den[:]), reads=["den"], writes=["den"])
            p.op("dve", lambda e, i=i: e.tensor_scalar(out=cw[:, i, :], in0=ex[:], scalar1=den[:, 0:1], scalar2=None, op0=ALU.mult), reads=["ex", "den"], writes=[f"cw{i}"])
            yield
    emit_norm_transpose(p, None, gains[:, 0:8], "gain0", bigA, "bigA", ident, NT, sc, xres=xres)
    wbufs = [(p.sb(f"wgb{j}", [128, 8, 256], BF16), p.sb(f"wub{j}", [128, 8, 256], BF16), p.sb(f"wdb{j}", [128, 2, 1024], BF16)) for j in range(2)]
    actTs = [p.sb(f"actT{i}", [128, 2, T], BF16) for i in range(2)]
    sgt = [p.sb(f"sgt{i}", [128, 512], BF16) for i in range(2)]
    emit_ffn(p, bigA, "bigA", xres, NT, [(mg[e_], mu[e_], md[e_], dfe) for e_ in range(nexp)], pm, wbufs, actTs, sgt, cw=cw, side=router_gen())
    ot = [rj, grb]
    otk = ["rj", "grb"]
    for i in range(NT):
        b = i % 2
        xi = xres[:, i, :]
        p.op("act", lambda e, xi=xi, b=b: e.activation(out=junk[:], in_=xi, func=AF.Square, accum_out=ss[b][:]), reads=[f"xres{i}"], writes=[f"{tag}_junk", f"{tag}_ss{b}"])
        p.op("act", lambda e, b=b: e.activation(out=rs[b][:], in_=ss[b][:], func=AF.Sqrt, scale=1.0 / D, bias=epsb[:, 0:1]), reads=[f"{tag}_ss{b}", "epsb"], writes=[f"{tag}_rs{b}"])
        p.op("dve", lambda e, b=b: e.reciprocal(out=rs[b][:], in_=rs[b][:]), reads=[f"{tag}_rs{b}"], writes=[f"{tag}_rs{b}"])
        p.op("dve", lambda e, xi=xi, b=b: e.scalar_tensor_tensor(out=ot[b][:], in0=xi, scalar=rs[b][:, 0:1], in1=gfb[:], op0=ALU.mult, op1=ALU.mult),
             reads=[f"xres{i}", f"{tag}_rs{b}", "gfb"], writes=[otk[b]])
        p.dma("sp", out[i * 128:(i + 1) * 128, :], ot[b][:], f"st_o{b}", reads=[otk[b]])
    return p.build() if own else p.phase_end()


def build_fused():
    p = Prog()
    LL, T = L, TPC
    NSUB = LL // T
    NT = T // 128
    ext = {}

    def EI(name, shape, dt=F32):
        ext[name] = p.dram(name, list(shape), dt, "ExternalInput").ap()
        return ext[name]

    def SC(name, shape, dt):
        return p.dram("sc_" + name, list(shape), dt, "Internal").ap()

    x = EI("x", [LL, D])
    padrow = EI("padrow", [1, LL], BF16)
    g_mix0 = EI("g_mix0", [128, 8]); w_in0 = EI("w_in0", [D, 2048])
    s5prm = EI("s5prm", [16, 128, 67]); s5dd = EI("s5dd", [16, 32, 1])
    wglu = EI("wglu", [512, 512]); bglu = EI("bglu", [128, 4]); w_out0 = EI("w_out0", [D, D]); g_ffn0 = EI("g_ffn0", [128, 8])
    wg = EI("wg", [D, DFF]); wu = EI("wu", [D, DFF]); wd = EI("wd", [DFF, D])
    g_mix1 = EI("g_mix1", [128, 8]); w_in1 = EI("w_in1", [D, 3088]); negb = EI("negb", [128, 1])
    w_out1 = EI("w_out1", [D, D]); g_ffn1 = EI("g_ffn1", [128, 8]); g_row1 = EI("g_row1", [1, D]); wrT = EI("wrT", [NEXP, D])
    mg = EI("mg", [NEXP, D, DFE]); mu = EI("mu", [NEXP, D, DFE]); md = EI("md", [NEXP, DFE, D]); gfin = EI("gfin", [1, D])
    out = p.dram("out", [T, D], F32, "ExternalOutput").ap()
    featT0 = SC("featT0", [1536, LL], BF16); v0 = SC("v0", [LL, 512], BF16)
    sbT = SC("sbT", [512, LL], BF16); ysT = SC("ysT", [512, LL], F32)
    feat1T = SC("feat1T", [2048, LL], BF16); v1 = SC("v1", [LL, 1024], BF16); flT = SC("flT", [16, LL], F32)
    x1 = SC("x1", [T, D], F32); coT = SC("coT", [D, T], BF16)

    p.io = dict(x=x, gain=g_mix0, w=w_in0, featT=featT0, v=v0)
    build_phase_a(nsub=NSUB, p=p)
    p.io = dict(qT=PairView(lambda h: featT0[512 + h * 64:512 + (h + 1) * 64, :]), kT=PairView(lambda h: featT0[1024 + h * 64:1024 + (h + 1) * 64, :]),
                v=PairView(lambda h: v0[:, h * 64:(h + 1) * 64]), oT=PairView(lambda h: sbT[h * 64:(h + 1) * 64, :]))
    p.io.update(uT=PairView(lambda u: featT0[u * 32:(u + 1) * 32, :]), prm=s5prm, dd=s5dd, yT=PairView(lambda u: ysT[u * 32:(u + 1) * 32, :]))
    psx = p.ps("psx", [128, 512], F32)
    gens = build_s5(16, LL, TC=512, p=p, psx=psx, defer=True)
    build_attn("sb", 8, LL, p=p, side=gens, side_every=(8, (0, 3, 6)), n_ps_s=3)
    p.io = dict(x=x, ysT=ysT, sbT=sbT, wglu=wglu, bglu=bglu, wout=w_out0, g_ffn=g_ffn0, wg=wg, wu=wu, wd=wd, g_mix=g_mix1, win=w_in1,
                x1=x1, featT=feat1T, v1=v1, flT=flT)
    build_phase_c(NT, nsub=NSUB, p=p)
    SEG = LL * 16 // 128
    p.io = dict(qT=PairView(lambda h: feat1T[h * 64:(h + 1) * 64, :]), kT=PairView(lambda h: feat1T[1024 + h * 64:1024 + (h + 1) * 64, :]),
                v=PairView(lambda h: v1[:, h * 64:(h + 1) * 64]), oT=PairView(lambda h: coT[h * 64:(h + 1) * 64, :]),
                fl=flT.rearrange("h (s f) -> (h s) f", f=SEG), negb=negb, padrow=padrow)
    nqc = LL // 512
    build_attn("fox", 16, LL, qcs=list(range(nqc - T // 512, nqc)), p=p)
    p.io = dict(x1=x1, coT=coT, wout=w_out1, g_ffn=g_ffn1, g_row=g_row1, wrT=wrT, mg=mg, mu=mu, md=md, gfin=gfin, out=out)
    build_phase_e(NT, p=p)
    return p.build()


def fused_inputs(inp, xb_pad, padrow):
    C = np.ascontiguousarray
    return dict(x=xb_pad, padrow=padrow, g_mix0=gainT_of(inp["ev_norm_mix"][0]), w_in0=inp["ev_w_in"][0],
                s5prm=np.stack([s5_prm_of(inp, gp) for gp in range(16)]),
                s5dd=C(np.stack([inp["s5_d"][0][2 * gp:2 * gp + 2].reshape(32, 1) for gp in range(16)]).astype(np.float32)),
                wglu=inp["s5_w_glu"][0], bglu=C(inp["s5_b_glu"][0].reshape(4, 128).T), w_out0=inp["ev_w_out"][0], g_ffn0=gainT_of(inp["ev_norm_ffn"][0]),
                wg=inp["ffn_w_gate"][0], wu=inp["ffn_w_up"][0], wd=inp["ffn_w_down"][0], g_mix1=gainT_of(inp["od_norm_mix"][0]), w_in1=inp["od_w_in"][0],
                negb=C(np.repeat(-inp["fox_b_f"][0].astype(np.float32), 8).reshape(128, 1)), w_out1=inp["od_w_out"][0],
                g_ffn1=gainT_of(inp["od_norm_ffn"][0]), g_row1=C(inp["od_norm_ffn"][0].reshape(1, D)), wrT=C(inp["moe_w_router"][0].T),
                mg=inp["moe_w_gate"][0], mu=inp["moe_w_up"][0], md=inp["moe_w_down"][0], gfin=C(inp["final_norm"].reshape(1, D)))


def kernel_fused(**inp):
    inp = {k: np.asarray(v) for k, v in inp.items()}
    x = inp["x"]
    maps = []
    for c in range(NCORES):
        b, r = divmod(c, 4)
        n_real = (r + 1) * TPC
        xp = np.zeros((L, D), np.float32)
        xp[L - n_real:] = x[b, :n_real]
        pr = np.zeros((1, L), NPBF)
        pr[0, :L - n_real] = -30000.0
        maps.append(fused_inputs(inp, xp, pr))
    res = run_spmd(_prog("FUSED", build_fused), maps)
    out = np.zeros((B, L, D), np.float32)
    for c in range(NCORES):
        b, r = divmod(c, 4)
        out[b, r * TPC:(r + 1) * TPC] = res[c]["out"]
    return out


def run_spmd(nc, in_maps):
    res = run_bass_kernel_spmd(nc, in_maps, core_ids=list(range(NCORES)))
    return res.results


def gainT_of(g):
    return np.ascontiguousarray(g.reshape(8, 128).T)


def s5_prm_of(inp, gp):
    g0 = 2 * gp
    are = inp["s5_a_re"][0][g0:g0 + 2].reshape(128, 1)
    aim = inp["s5_a_im"][0][g0:g0 + 2].reshape(128, 1)
    ls = np.repeat(inp["s5_log_step"][0][g0:g0 + 2], 64).reshape(128, 1)
    bre = inp["s5_b_re"][0][g0:g0 + 2].reshape(128, 16)
    bim = inp["s5_b_im"][0][g0:g0 + 2].reshape(128, 16)
    cre = inp["s5_c_re"][0][g0:g0 + 2].transpose(0, 2, 1).reshape(128, 16)
    cim = inp["s5_c_im"][0][g0:g0 + 2].transpose(0, 2, 1).reshape(128, 16)
    return np.ascontiguousarray(np.concatenate([are, aim, ls, bre, bim, cre, cim], 1), dtype=np.float32)


_CACHE = {}


def _prog(name, fn):
    if name not in _CACHE:
        _CACHE[name] = fn()
    return _CACHE[name]


def kernel_unfused(**inp):
    inp = {k: np.asarray(v) for k, v in inp.items()}
    C = np.ascontiguousarray
    x = inp["x"].reshape(B * L, D)
    ra = run_spmd(_prog("A", build_phase_a), [{"x": C(x[c * TPC:(c + 1) * TPC]), "gain": gainT_of(inp["ev_norm_mix"][0]), "w": inp["ev_w_in"][0]}
                                              for c in range(NCORES)])
    featT = np.concatenate([r["featT"] for r in ra], axis=1)
    v0 = np.concatenate([r["v"] for r in ra], axis=0)
    maps = []
    for c in range(NCORES):
        prs = [divmod(2 * c + j, 8) for j in range(2)]
        maps.append({"qT": C(np.stack([featT[512 + h * 64:512 + (h + 1) * 64, b * L:(b + 1) * L] for b, h in prs])),
                     "kT": C(np.stack([featT[1024 + h * 64:1024 + (h + 1) * 64, b * L:(b + 1) * L] for b, h in prs])),
                     "v": C(np.stack([v0[b * L:(b + 1) * L, h * 64:(h + 1) * 64] for b, h in prs]))})
    rb1 = run_spmd(_prog("SB", lambda: build_attn("sb", 2)), maps)
    sbT = np.zeros((B, 512, L), NPBF)
    for c in range(NCORES):
        for j in range(2):
            b, h = divmod(2 * c + j, 8)
            sbT[b, h * 64:(h + 1) * 64] = rb1[c]["oT"][j]
    maps = []
    for c in range(NCORES):
        us = [divmod(4 * c + j, 16) for j in range(4)]
        maps.append({"uT": C(np.stack([featT[gp * 32:(gp + 1) * 32, b * L:(b + 1) * L] for b, gp in us])),
                     "prm": np.stack([s5_prm_of(inp, gp) for b, gp in us]),
                     "dd": C(np.stack([inp["s5_d"][0][2 * gp:2 * gp + 2].reshape(32, 1) for b, gp in us]).astype(np.float32))})
    rb2 = run_spmd(_prog("S5", lambda: build_s5(4)), maps)
    ysT = np.zeros((B, 512, L), np.float32)
    for c in range(NCORES):
        for j in range(4):
            b, gp = divmod(4 * c + j, 16)
            ysT[b, gp * 32:(gp + 1) * 32] = rb2[c]["yT"][j]
    maps = []
    for c in range(NCORES):
        b, t0 = divmod(c * TPC, L)
        maps.append(dict(x=C(x[c * TPC:(c + 1) * TPC]), ysT=C(ysT[b][:, t0:t0 + TPC]), sbT=C(sbT[b][:, t0:t0 + TPC]),
                         wglu=inp["s5_w_glu"][0], bglu=C(inp["s5_b_glu"][0].reshape(4, 128).T), wout=inp["ev_w_out"][0],
                         g_ffn=gainT_of(inp["ev_norm_ffn"][0]), wg=inp["ffn_w_gate"][0], wu=inp["ffn_w_up"][0], wd=inp["ffn_w_down"][0],
                         g_mix=gainT_of(inp["od_norm_mix"][0]), win=inp["od_w_in"][0]))
    rc = run_spmd(_prog("C", build_phase_c), maps)
    x1 = [r["x1"] for r in rc]
    feat1 = np.concatenate([r["featT"] for r in rc], axis=1)
    v1 = np.concatenate([r["v1"] for r in rc], axis=0)
    fl = np.concatenate([r["flT"] for r in rc], axis=1)
    maps = []
    for c in range(NCORES):
        prs = [divmod(4 * c + j, 16) for j in range(4)]
        maps.append({"qT": C(np.stack([feat1[h * 64:(h + 1) * 64, b * L:(b + 1) * L] for b, h in prs])),
                     "kT": C(np.stack([feat1[1024 + h * 64:1024 + (h + 1) * 64, b * L:(b + 1) * L] for b, h in prs])),
                     "v": C(np.stack([v1[b * L:(b + 1) * L, h * 64:(h + 1) * 64] for b, h in prs])),
                     "fl": C(np.stack([fl[h, b * L:(b + 1) * L] for b, h in prs]).reshape(128, -1)),
                     "negb": C(np.repeat(np.array([-inp["fox_b_f"][0][h] for b, h in prs], np.float32), 32).reshape(128, 1)),
                     "padrow": np.zeros((1, L), NPBF)})
    rd = run_spmd(_prog("FOX", lambda: build_attn("fox", 4)), maps)
    coT = np.zeros((B, 1024, L), NPBF)
    for c in range(NCORES):
        for j in range(4):
            b, h = divmod(4 * c + j, 16)
            coT[b, h * 64:(h + 1) * 64] = rd[c]["oT"][j]
    maps = []
    for c in range(NCORES):
        b, t0 = divmod(c * TPC, L)
        maps.append(dict(x1=x1[c], coT=C(coT[b][:, t0:t0 + TPC]), wout=inp["od_w_out"][0], g_ffn=gainT_of(inp["od_norm_ffn"][0]),
                         g_row=C(inp["od_norm_ffn"][0].reshape(1, D)), wrT=C(inp["moe_w_router"][0].T), mg=inp["moe_w_gate"][0],
                         mu=inp["moe_w_up"][0], md=inp["moe_w_down"][0], gfin=C(inp["final_norm"].reshape(1, D))))
    re_ = run_spmd(_prog("E", build_phase_e), maps)
    out = np.concatenate([r["out"] for r in re_], axis=0).reshape(B, L, D)
    return out.astype(np.float32)


def kernel(**inp):
    return kernel_fused(**inp)
```
